# Optimizing a Trainium2 kernel written in Bass

```python
import math
import jax, jax.numpy as jnp
from jax import lax
import numpy as np

D_MODEL = 1024
BATCH = 8
SEQ = 4096
DEPTH = 4

N_MIXERS = 4
N_RET = (DEPTH + 3) // 4
N_CONV = (DEPTH + 2) // 4
N_NSA = (DEPTH + 1) // 4
N_POOL = DEPTH // 4

ROPE_THETA = 10000.0
DN_ALPHA = (2.0 * DEPTH) ** 0.25
DN_BETA = (8.0 * DEPTH) ** -0.25
LN_EPS = 1e-5
NEG = -1e30

RET_HEADS = 4
RET_DK = D_MODEL // RET_HEADS
RET_DV = 2 * D_MODEL // RET_HEADS
RET_CHUNK = 128

CONV_WIDTH = 31

NSA_HEADS = 16
NSA_KV_GROUPS = 2
NSA_HEAD_DIM = D_MODEL // NSA_HEADS
NSA_CMP_BLOCK = 32
NSA_CMP_STRIDE = 16
NSA_CMP_HIDDEN = 256
NSA_SEL_BLOCK = 64
NSA_TOP_N = 16
NSA_WINDOW = 512
NSA_Q_BLOCK = 64
NSA_FORCE = 1e6
NSA_IN_WIDTH = NSA_HEADS * NSA_HEAD_DIM + 6 * NSA_KV_GROUPS * NSA_HEAD_DIM + 3 * NSA_HEADS

POOL_WINDOWS = (2, 4, 8, 16)
POOL_GROUP = D_MODEL // len(POOL_WINDOWS)

N_EXPERTS = 32
TOP_K = 4
D_FF = D_MODEL
SWIGLU_LIMIT = 7.0
SWIGLU_ALPHA = 1.702
MOE_BLOCK = 256

kernel_name = 'hybrid_ret_conv_nsa_pool_moe_trunk'


def layer_norm(x, g, b):
    xf = x.astype(jnp.float32)
    mu = xf.mean(-1, keepdims=True)
    var = jnp.square(xf - mu).mean(-1, keepdims=True)
    return ((xf - mu) * lax.rsqrt(var + LN_EPS)).astype(x.dtype) * g + b


def rope(x, positions):
    d = x.shape[-1]
    inv = ROPE_THETA ** (-jnp.arange(0, d, 2, dtype=jnp.float32) / d)
    ang = positions.astype(jnp.float32)[:, :, None, None] * inv
    cos, sin = jnp.cos(ang).astype(x.dtype), jnp.sin(ang).astype(x.dtype)
    x1, x2 = x[..., : d // 2], x[..., d // 2:]
    return jnp.concatenate([x1 * cos - x2 * sin, x1 * sin + x2 * cos], axis=-1)


def retention_mixer(h, positions, w_in, gn_g, gn_b, w_out):
    B, S, _ = h.shape
    H, dk, dv, C = RET_HEADS, RET_DK, RET_DV, RET_CHUNK
    n = S // C
    dt = h.dtype
    q, k, v, g = jnp.split(h @ w_in, [H * dk, 2 * H * dk, 2 * H * dk + H * dv], axis=-1)
    q = rope(q.reshape(B, S, H, dk), positions)
    k = rope(k.reshape(B, S, H, dk), positions) * (dk ** -0.5)
    v = v.reshape(B, S, H, dv)
    log_gamma = jnp.log1p(-(2.0 ** (-5.0 - jnp.arange(H, dtype=jnp.float32))))
    i = jnp.arange(C, dtype=jnp.float32)
    rel = i[:, None] - i[None, :]
    decay_intra = jnp.where(rel >= 0, jnp.exp(log_gamma[:, None, None] * jnp.maximum(rel, 0.0)), 0.0).astype(dt)
    decay_q = jnp.exp(log_gamma[:, None] * (i + 1.0)).astype(dt)[..., None]
    decay_k = jnp.exp(log_gamma[:, None] * (C - 1.0 - i)).astype(dt)[..., None]
    decay_chunk = jnp.exp(log_gamma * C).astype(dt)[:, None, None]

    def chunks(t):
        return t.reshape(B, n, C, H, t.shape[-1]).transpose(1, 0, 3, 2, 4)

    def step(state, qkv):
        qc, kc, vc = qkv
        o = jnp.einsum('bhij,bhjv->bhiv', jnp.einsum('bhid,bhjd->bhij', qc, kc) * decay_intra, vc)
        o = o + jnp.einsum('bhid,bhdv->bhiv', qc * decay_q, state)
        state = state * decay_chunk + jnp.einsum('bhjd,bhjv->bhdv', kc * decay_k, vc)
        return state, o

    _, o = lax.scan(step, jnp.zeros((B, H, dk, dv), dt), (chunks(q), chunks(k), chunks(v)))
    o = o.transpose(1, 0, 3, 2, 4).reshape(B, S, H, dv)
    of = o.astype(jnp.float32)
    mu = of.mean(-1, keepdims=True)
    var = jnp.square(of - mu).mean(-1, keepdims=True)
    o = ((of - mu) * lax.rsqrt(var + LN_EPS)).astype(dt).reshape(B, S, H * dv) * gn_g + gn_b
    return (jax.nn.silu(g) * o) @ w_out


def conv_mixer(h, w_pw1, b_pw1, w_dw, b_dw, ln_g, ln_b, w_pw2, b_pw2):
    a, gate = jnp.split(h @ w_pw1 + b_pw1, 2, axis=-1)
    u = a * jax.nn.sigmoid(gate)
    u = jnp.pad(u, ((0, 0), (CONV_WIDTH - 1, 0), (0, 0)))
    u = lax.conv_general_dilated(u, w_dw[:, None, :], window_strides=(1,), padding='VALID',
                                 dimension_numbers=('NWC', 'WIO', 'NWC'),
                                 feature_group_count=D_MODEL) + b_dw
    u = jax.nn.silu(layer_norm(u, ln_g, ln_b))
    return u @ w_pw2 + b_pw2


def _compress(t, pos_emb, w1, w2, win_idx):
    blk = t[:, win_idx] + pos_emb[:, None, :]
    b_, n, l, g_, d = blk.shape
    blk = blk.transpose(0, 3, 1, 2, 4).reshape(b_, g_, n, l * d)
    return jax.nn.gelu(blk @ w1) @ w2


def nsa_mixer(h, positions, w_in, gate_b, cmp_pos_k, cmp_pos_v, cmp_k_w1, cmp_k_w2,
              cmp_v_w1, cmp_v_w2, w_out):
    B, S, _ = h.shape
    H, G, dh = NSA_HEADS, NSA_KV_GROUPS, NSA_HEAD_DIM
    R = H // G
    Qb, W, ls = NSA_Q_BLOCK, NSA_WINDOW, NSA_SEL_BLOCK
    dt = h.dtype
    sizes = [H * dh] + [G * dh] * 6 + [3 * H]
    q, kc, vc, ks, vs, kw, vw, gl = jnp.split(h @ w_in, np.cumsum(sizes)[:-1].tolist(), axis=-1)
    q = rope(q.reshape(B, S, H, dh), positions) * (dh ** -0.5)
    kc = rope(kc.reshape(B, S, G, dh), positions)
    ks = rope(ks.reshape(B, S, G, dh), positions)
    kw = rope(kw.reshape(B, S, G, dh), positions)
    vc, vs, vw = (t.reshape(B, S, G, dh) for t in (vc, vs, vw))
    gates = jax.nn.sigmoid(gl + gate_b).reshape(B, S, H, 3)

    n_cmp = (S - NSA_CMP_BLOCK) // NSA_CMP_STRIDE + 1
    cmp_start = np.arange(n_cmp) * NSA_CMP_STRIDE
    win_idx = cmp_start[:, None] + np.arange(NSA_CMP_BLOCK)[None, :]
    k_cmp = _compress(kc, cmp_pos_k, cmp_k_w1, cmp_k_w2, win_idx)
    v_cmp = _compress(vc, cmp_pos_v, cmp_v_w1, cmp_v_w2, win_idx)
    cmp_end = jnp.asarray(cmp_start + NSA_CMP_BLOCK - 1)
    n_sel = S // ls
    j_np = np.arange(n_sel)
    overlap = jnp.asarray(((cmp_start[:, None] < (j_np[None, :] + 1) * ls) &
                           (cmp_start[:, None] + NSA_CMP_BLOCK > j_np[None, :] * ls)).astype(np.float32))
    k_sel_n = min(NSA_TOP_N, n_sel)
    ks_blocks = ks.reshape(B, n_sel, ls, G, dh).transpose(0, 3, 1, 2, 4)
    vs_blocks = vs.reshape(B, n_sel, ls, G, dh).transpose(0, 3, 1, 2, 4)
    kw_pad = jnp.pad(kw, ((0, 0), (W, 0), (0, 0), (0, 0)))
    vw_pad = jnp.pad(vw, ((0, 0), (W, 0), (0, 0), (0, 0)))
    b_ix = jnp.arange(B)[:, None, None, None]
    g_ix = jnp.arange(G)[None, :, None, None]
    j = jnp.arange(n_sel)

    def q_block(qb):
        t0 = qb * Qb
        t = t0 + jnp.arange(Qb)
        qi = lax.dynamic_slice_in_dim(q, t0, Qb, axis=1).reshape(B, Qb, G, R, dh)
        s_c = jnp.einsum('bqgrd,bgnd->bgrqn', qi, k_cmp).astype(jnp.float32)
        mask_c = cmp_end[None, :] <= t[:, None]
        p_c = jnp.where(mask_c, jax.nn.softmax(jnp.where(mask_c, s_c, NEG), axis=-1), 0.0)
        o_c = jnp.einsum('bgrqn,bgnd->bqgrd', p_c.astype(dt), v_cmp)
        imp = jnp.einsum('bgrqn,nj->bgqj', p_c, overlap)
        cur = t // ls
        forced = (j[None, :] == 0) | (j[None, :] == cur[:, None]) | (j[None, :] == cur[:, None] - 1)
        score = jnp.where(j[None, :] * ls <= t[:, None], jnp.where(forced, NSA_FORCE, imp), NEG)
        top_s, top_i = lax.top_k(score, k_sel_n)
        k_g = ks_blocks[b_ix, g_ix, top_i]
        v_g = vs_blocks[b_ix, g_ix, top_i]
        key_pos = top_i[..., None] * ls + jnp.arange(ls)
        mask_s = (top_s > 0.5 * NEG)[..., None] & (key_pos <= t[None, None, :, None, None])
        s_s = jnp.einsum('bqgrd,bgqnld->bgrqnl', qi, k_g).astype(jnp.float32)
        s_s = jnp.where(mask_s[:, :, None], s_s, NEG)
        p_s = jax.nn.softmax(s_s.reshape(B, G, R, Qb, k_sel_n * ls), axis=-1).reshape(s_s.shape)
        o_s = jnp.einsum('bgrqnl,bgqnld->bqgrd', p_s.astype(dt), v_g)
        kwin = lax.dynamic_slice_in_dim(kw_pad, t0, Qb + W, axis=1)
        vwin = lax.dynamic_slice_in_dim(vw_pad, t0, Qb + W, axis=1)
        kpos = t0 - W + jnp.arange(Qb + W)
        mask_w = (kpos[None, :] <= t[:, None]) & (kpos[None, :] > t[:, None] - W) & (kpos[None, :] >= 0)
        s_w = jnp.einsum('bqgrd,bkgd->bgrqk', qi, kwin).astype(jnp.float32)
        p_w = jax.nn.softmax(jnp.where(mask_w, s_w, NEG), axis=-1)
        o_w = jnp.einsum('bgrqk,bkgd->bqgrd', p_w.astype(dt), vwin)
        gt = lax.dynamic_slice_in_dim(gates, t0, Qb, axis=1).reshape(B, Qb, G, R, 3)
        o = gt[..., 0:1] * o_c + gt[..., 1:2] * o_s + gt[..., 2:3] * o_w
        return o.reshape(B, Qb, H * dh)

    out = lax.map(q_block, jnp.arange(S // Qb))
    out = out.transpose(1, 0, 2, 3).reshape(B, S, H * dh)
    return out @ w_out


def pool_mixer(h, w, b, scale):
    B, S, D = h.shape
    cs = jnp.pad(jnp.cumsum(h.astype(jnp.float32), axis=1), ((0, 0), (1, 0), (0, 0)))
    t = jnp.arange(S)
    groups = []
    for gi, win in enumerate(POOL_WINDOWS):
        sl = slice(gi * POOL_GROUP, (gi + 1) * POOL_GROUP)
        lo = jnp.maximum(t + 1 - win, 0)
        cnt = (t + 1 - lo).astype(jnp.float32)[None, :, None]
        groups.append((cs[:, 1:, sl] - cs[:, lo, sl]) / cnt)
    pooled = jnp.concatenate(groups, axis=-1).astype(h.dtype) - h
    y = jnp.einsum('bsgc,gcd->bsgd', pooled.reshape(B, S, len(POOL_WINDOWS), POOL_GROUP), w)
    return (y.reshape(B, S, D) + b) * scale


def moe(h, router_w, router_b, w1, b1, w2, b2):
    B, S, D = h.shape
    T = B * S
    xf = h.reshape(T, D)
    logits = (xf @ router_w + router_b).astype(jnp.float32)
    top_v, top_i = lax.top_k(logits, TOP_K)
    gate = jax.nn.softmax(top_v, axis=-1).astype(h.dtype)
    flat_e = top_i.reshape(-1)
    flat_tok = jnp.arange(T * TOP_K, dtype=jnp.int32) // TOP_K
    flat_w = gate.reshape(-1)
    order = jnp.argsort(flat_e)
    e_sorted = flat_e[order]
    counts = jnp.bincount(flat_e, length=N_EXPERTS)
    starts = jnp.cumsum(counts) - counts
    blocks_per = (counts + MOE_BLOCK - 1) // MOE_BLOCK
    block_end = jnp.cumsum(blocks_per)
    pad_start = (block_end - blocks_per) * MOE_BLOCK
    dest = pad_start[e_sorted] + (jnp.arange(T * TOP_K) - starts[e_sorted])
    n_blocks = -(-(T * TOP_K) // MOE_BLOCK) + N_EXPERTS
    slot_tok = jnp.full((n_blocks * MOE_BLOCK,), T, jnp.int32).at[dest].set(flat_tok[order])
    slot_w = jnp.zeros((n_blocks * MOE_BLOCK,), h.dtype).at[dest].set(flat_w[order])
    block_expert = jnp.minimum(jnp.searchsorted(block_end, jnp.arange(n_blocks), side='right'), N_EXPERTS - 1)
    x_pad = jnp.concatenate([xf, jnp.zeros((1, D), h.dtype)], axis=0)

    def expert_block(args):
        tok, e = args
        gu = x_pad[tok] @ w1[e] + b1[e]
        g_, up = gu[:, :D_FF], gu[:, D_FF:]
        g_ = jnp.minimum(g_, SWIGLU_LIMIT)
        up = jnp.clip(up, -SWIGLU_LIMIT, SWIGLU_LIMIT)
        return ((up + 1.0) * (g_ * jax.nn.sigmoid(SWIGLU_ALPHA * g_))) @ w2[e] + b2[e]

    out = lax.map(expert_block, (slot_tok.reshape(n_blocks, MOE_BLOCK), block_expert))
    y = jnp.zeros((T + 1, D), h.dtype).at[slot_tok].add(out.reshape(-1, D) * slot_w[:, None])
    return y[:T].reshape(B, S, D)


def _w(key, shape, fan_in, gain=1.0):
    return jax.random.normal(key, shape, jnp.float32) * (gain * fan_in ** -0.5)


def _gain(key, shape):
    return 1.0 + 0.02 * jax.random.normal(key, shape, jnp.float32)


def _bias(key, shape, s=0.02):
    return s * jax.random.normal(key, shape, jnp.float32)


def setup_inputs(seed: int = 0) -> dict:
    key = jax.random.key(seed)
    ks = iter(jax.random.split(key, 64))
    D, E, F = D_MODEL, N_EXPERTS, D_FF
    H, dk, dv = RET_HEADS, RET_DK, RET_DV
    Hn, G, dh = NSA_HEADS, NSA_KV_GROUPS, NSA_HEAD_DIM
    offs = jax.random.randint(next(ks), (BATCH, 1), 0, 1024, dtype=jnp.int32)
    return {
        'x': jax.random.normal(next(ks), (BATCH, SEQ, D), jnp.float32),
        'c': jax.random.normal(next(ks), (BATCH, D), jnp.float32),
        'positions': offs + jnp.arange(SEQ, dtype=jnp.int32)[None, :],
        'ada_w': _w(next(ks), (DEPTH, D, 6 * D), D, 0.2),
        'ada_b': _bias(next(ks), (DEPTH, 6 * D), 0.01),
        'ln1_g': _gain(next(ks), (DEPTH, D)),
        'ln1_b': _bias(next(ks), (DEPTH, D)),
        'ln2_g': _gain(next(ks), (DEPTH, D)),
        'ln2_b': _bias(next(ks), (DEPTH, D)),
        'router_w': _w(next(ks), (DEPTH, D, E), D),
        'router_b': _bias(next(ks), (DEPTH, E), 0.01),
        'moe_w1': _w(next(ks), (DEPTH, E, D, 2 * F), D),
        'moe_b1': _bias(next(ks), (DEPTH, E, 2 * F), 0.01),
        'moe_w2': _w(next(ks), (DEPTH, E, F, D), F, DN_BETA),
        'moe_b2': _bias(next(ks), (DEPTH, E, D), 0.01),
        'ret_w_in': _w(next(ks), (N_RET, D, 2 * H * dk + 2 * H * dv), D),
        'ret_gn_g': _gain(next(ks), (N_RET, H * dv)),
        'ret_gn_b': _bias(next(ks), (N_RET, H * dv)),
        'ret_w_out': _w(next(ks), (N_RET, H * dv, D), H * dv, DN_BETA),
        'conv_w_pw1': _w(next(ks), (N_CONV, D, 2 * D), D),
        'conv_b_pw1': _bias(next(ks), (N_CONV, 2 * D)),
        'conv_w_dw': _w(next(ks), (N_CONV, CONV_WIDTH, D), CONV_WIDTH),
        'conv_b_dw': _bias(next(ks), (N_CONV, D)),
        'conv_ln_g': _gain(next(ks), (N_CONV, D)),
        'conv_ln_b': _bias(next(ks), (N_CONV, D)),
        'conv_w_pw2': _w(next(ks), (N_CONV, D, D), D, DN_BETA),
        'conv_b_pw2': _bias(next(ks), (N_CONV, D)),
        'nsa_w_in': _w(next(ks), (N_NSA, D, NSA_IN_WIDTH), D),
        'nsa_gate_b': _bias(next(ks), (N_NSA, 3 * Hn), 0.1),
        'nsa_cmp_pos_k': _bias(next(ks), (N_NSA, NSA_CMP_BLOCK, dh), 0.1),
        'nsa_cmp_pos_v': _bias(next(ks), (N_NSA, NSA_CMP_BLOCK, dh), 0.1),
        'nsa_cmp_k_w1': _w(next(ks), (N_NSA, NSA_CMP_BLOCK * dh, NSA_CMP_HIDDEN), NSA_CMP_BLOCK * dh),
        'nsa_cmp_k_w2': _w(next(ks), (N_NSA, NSA_CMP_HIDDEN, dh), NSA_CMP_HIDDEN),
        'nsa_cmp_v_w1': _w(next(ks), (N_NSA, NSA_CMP_BLOCK * dh, NSA_CMP_HIDDEN), NSA_CMP_BLOCK * dh),
        'nsa_cmp_v_w2': _w(next(ks), (N_NSA, NSA_CMP_HIDDEN, dh), NSA_CMP_HIDDEN),
        'nsa_w_out': _w(next(ks), (N_NSA, Hn * dh, D), Hn * dh, DN_BETA),
        'pool_w': _w(next(ks), (N_POOL, len(POOL_WINDOWS), POOL_GROUP, POOL_GROUP), POOL_GROUP, DN_BETA),
        'pool_b': _bias(next(ks), (N_POOL, D)),
        'pool_scale': _gain(next(ks), (N_POOL, D)),
    }


def reference(x, c, positions, ada_w, ada_b, ln1_g, ln1_b, ln2_g, ln2_b, router_w, router_b,
              moe_w1, moe_b1, moe_w2, moe_b2, ret_w_in, ret_gn_g, ret_gn_b, ret_w_out,
              conv_w_pw1, conv_b_pw1, conv_w_dw, conv_b_dw, conv_ln_g, conv_ln_b, conv_w_pw2,
              conv_b_pw2, nsa_w_in, nsa_gate_b, nsa_cmp_pos_k, nsa_cmp_pos_v, nsa_cmp_k_w1,
              nsa_cmp_k_w2, nsa_cmp_v_w1, nsa_cmp_v_w2, nsa_w_out, pool_w, pool_b, pool_scale):
    c_act = jax.nn.silu(c)
    for i in range(DEPTH):
        kind, j = i % N_MIXERS, i // N_MIXERS
        mod = c_act @ ada_w[i] + ada_b[i]
        sh1, sc1, g1, sh2, sc2, g2 = (m[:, None, :] for m in jnp.split(mod, 6, axis=-1))
        h = x * (1.0 + sc1) + sh1
        if kind == 0:
            y = retention_mixer(h, positions, ret_w_in[j], ret_gn_g[j], ret_gn_b[j], ret_w_out[j])
        elif kind == 1:
            y = conv_mixer(h, conv_w_pw1[j], conv_b_pw1[j], conv_w_dw[j], conv_b_dw[j],
                           conv_ln_g[j], conv_ln_b[j], conv_w_pw2[j], conv_b_pw2[j])
        elif kind == 2:
            y = nsa_mixer(h, positions, nsa_w_in[j], nsa_gate_b[j], nsa_cmp_pos_k[j], nsa_cmp_pos_v[j],
                          nsa_cmp_k_w1[j], nsa_cmp_k_w2[j], nsa_cmp_v_w1[j], nsa_cmp_v_w2[j], nsa_w_out[j])
        else:
            y = pool_mixer(h, pool_w[j], pool_b[j], pool_scale[j])
        x = layer_norm(DN_ALPHA * x + (1.0 + g1) * y, ln1_g[i], ln1_b[i])
        h = x * (1.0 + sc2) + sh2
        y = moe(h, router_w[i], router_b[i], moe_w1[i], moe_b1[i], moe_w2[i], moe_b2[i])
        x = layer_norm(DN_ALPHA * x + (1.0 + g2) * y, ln2_g[i], ln2_b[i])
    return x
```

```python
import numpy as np
from contextlib import ExitStack
import concourse.bass as bass
import concourse.mybir as mybir
from concourse.bass_utils import run_bass_kernel_spmd

F32 = mybir.dt.float32
BF16 = mybir.dt.bfloat16
I32 = mybir.dt.int32
AF = mybir.ActivationFunctionType
ALU = mybir.AluOpType
AX = mybir.AxisListType


class Buf:
    def __init__(self, name, t=None):
        self.name = name
        self.t = t
        self.w = {}
        self.r = {}
        self.dsem = None

    def __getitem__(self, idx):
        return V(self, self.t[idx])


class V:
    def __init__(self, buf, ap):
        self.buf = buf
        self.ap = ap

    def __getitem__(self, idx):
        return V(self.buf, self.ap[idx])

    def rearrange(self, *a, **kw):
        return V(self.buf, self.ap.rearrange(*a, **kw))


class Eng:
    def __init__(self, name, e, sem):
        self.name = name
        self.e = e
        self.sem = sem
        self.cnt = 0
        self.waited = {}


class K:
    def __init__(self, nc, es):
        self.nc = nc
        self.es = es
        self.eng = {}
        for n in ("tensor", "vector", "scalar", "gpsimd", "sync"):
            sem = es.enter_context(nc.semaphore("c_" + n))
            self.eng[n] = Eng(n, getattr(nc, n), sem)
        self.dcnt = {}
        self.free_dsems = []
        self.pstack = []
        self.nsem = 5
        self.nbuf = 0
        self.ninst = 0

    def sb(self, name, shape, dtype):
        es = self.pstack[-1][0] if self.pstack else self.es
        self.nbuf += 1
        t = es.enter_context(self.nc.sbuf_tensor("%s_%d" % (name, self.nbuf), list(shape), dtype))
        b = Buf(name, t)
        if self.pstack:
            self.pstack[-1][1].append(b)
        return b

    def ps(self, name, shape, dtype=F32):
        t = self.es.enter_context(self.nc.psum_tensor(name, list(shape), dtype))
        return Buf(name, t)

    def begin_phase(self):
        pes = ExitStack()
        pes.__enter__()
        self.pstack.append((pes, []))

    def end_phase(self):
        self.barrier()
        pes, bufs = self.pstack.pop()
        for b in bufs:
            if b.dsem is not None:
                self.free_dsems.append(b.dsem)
                b.dsem = None
        pes.__exit__(None, None, None)

    def barrier(self):
        deps = [(e.sem, e.cnt) for e in self.eng.values() if e.cnt > 0]
        deps += [(v[0], v[1]) for v in self.dcnt.values() if v[1] > 0]
        for e in self.eng.values():
            self._wait(e, deps)

    def dram(self, name, shape, dtype, kind="Internal"):
        t = self.nc.dram_tensor(name, list(shape), dtype, kind=kind)
        b = Buf(name, t.ap())
        b.is_dram = True
        return b

    def region(self, name):
        return Buf(name, None)

    def _wait(self, eng, deps):
        for sem, val in deps:
            key = sem.name if hasattr(sem, "name") else id(sem)
            if key in self.dcnt:
                val = max(val, self.dcnt[key][1])
            if eng.waited.get(key, 0) < val:
                eng.e.wait_ge(sem, val)
                eng.waited[key] = val
                self.ninst += 1

    @staticmethod
    def _key(sem):
        return sem.name if hasattr(sem, "name") else id(sem)

    def op(self, en, method, **kw):
        eng = self.eng[en]
        reads, writes, args = [], [], {}
        for k, v in kw.items():
            if isinstance(v, V):
                (writes if k in ("out", "accum_out", "ap") else reads).append(v.buf)
                args[k] = v.ap
            else:
                args[k] = v
        deps = []
        for b in reads:
            deps += list(b.w.values())
        for b in writes:
            deps += list(b.w.values()) + list(b.r.values())
        if en == "tensor":
            deps = [d for d in deps if d[0] is not eng.sem]
        self._wait(eng, deps)
        ins = getattr(eng.e, method)(**args)
        eng.cnt += 1
        ins.then_inc(eng.sem, 1)
        self.ninst += 1
        rec = (eng.sem, eng.cnt)
        key = self._key(eng.sem)
        for b in reads:
            b.r[key] = rec
        for b in writes:
            b.w = {key: rec}
            b.r = {}
        return ins

    def dma(self, qn, out, in_, sbside=None, **kw):
        eng = self.eng[qn]
        deps = list(in_.buf.w.values()) + list(out.buf.w.values()) + list(out.buf.r.values())
        self._wait(eng, deps)
        if sbside is None:
            sbside = in_.buf if _is_dram(out) else out.buf
        if sbside.dsem is None:
            if self.free_dsems:
                sbside.dsem = self.free_dsems.pop()
            else:
                self.nsem += 1
                sbside.dsem = self.es.enter_context(self.nc.semaphore("d_%d" % self.nsem))
                self.dcnt[self._key(sbside.dsem)] = [sbside.dsem, 0]
        key = self._key(sbside.dsem)
        ins = eng.e.dma_start(out=out.ap, in_=in_.ap, **kw)
        self.dcnt[key][1] += 16
        ins.then_inc(sbside.dsem, 16)
        self.ninst += 1
        rec = (sbside.dsem, self.dcnt[key][1])
        in_.buf.r[key] = rec
        out.buf.w = {key: rec}
        out.buf.r = {}
        return ins

    def finish(self, bufs):
        eng = self.eng["sync"]
        deps = []
        for b in bufs:
            deps += list(b.w.values()) + list(b.r.values())
        self._wait(eng, deps)


def _is_dram(v):
    return getattr(v.buf, "is_dram", False)
D = 1024
S = 4096
NT = 32
DEPTH = 4
ALPHA_DN = (2.0 * DEPTH) ** 0.25
LN_EPS = 1e-5
NE = 32
PARAM_SHAPES = {
    'ada_w': (4, 1024, 6144), 'ada_b': (4, 6144), 'ln1_g': (4, 1024), 'ln1_b': (4, 1024),
    'ln2_g': (4, 1024), 'ln2_b': (4, 1024), 'router_w': (4, 1024, 32), 'router_b': (4, 32),
    'moe_w1': (4, 32, 1024, 2048), 'moe_b1': (4, 32, 2048), 'moe_w2': (4, 32, 1024, 1024),
    'moe_b2': (4, 32, 1024), 'ret_w_in': (1, 1024, 6144), 'ret_gn_g': (1, 2048),
    'ret_gn_b': (1, 2048), 'ret_w_out': (1, 2048, 1024), 'conv_w_pw1': (1, 1024, 2048),
    'conv_b_pw1': (1, 2048), 'conv_w_dw': (1, 31, 1024), 'conv_b_dw': (1, 1024),
    'conv_ln_g': (1, 1024), 'conv_ln_b': (1, 1024), 'conv_w_pw2': (1, 1024, 1024),
    'conv_b_pw2': (1, 1024), 'nsa_w_in': (1, 1024, 1840), 'nsa_gate_b': (1, 48),
    'nsa_cmp_pos_k': (1, 32, 64), 'nsa_cmp_pos_v': (1, 32, 64), 'nsa_cmp_k_w1': (1, 2048, 256),
    'nsa_cmp_k_w2': (1, 256, 64), 'nsa_cmp_v_w1': (1, 2048, 256), 'nsa_cmp_v_w2': (1, 256, 64),
    'nsa_w_out': (1, 1024, 1024), 'pool_w': (1, 4, 256, 256), 'pool_b': (1, 1024),
    'pool_scale': (1, 1024),
}


class Ctx:
    pass


def bcast(buf, ap1d, n=128):
    return V(buf, ap1d.partition_broadcast(n))


def colview(buf, ap1d):
    return V(buf, ap1d.rearrange("(c p) -> p c", p=128))


def setup(C):
    k = C.k
    C.P = {n: k.dram(n, list(s), F32, kind="ExternalInput") for n, s in PARAM_SHAPES.items()}
    C.x_in = k.dram("x", [S, D], F32, kind="ExternalInput")
    C.c_in = k.dram("c", [D], F32, kind="ExternalInput")
    C.pos_in = k.dram("positions", [S], I32, kind="ExternalInput")
    C.out = k.dram("out", [S, D], F32, kind="ExternalOutput")
    C.xs = [k.dram("xs0", [S, D], F32), k.dram("xs1", [S, D], F32)]
    C.mod_d = k.dram("mod_d", [4, 6144], F32)
    C.cst = {}
    for n, (shape, dt) in CONST_SPECS.items():
        C.cst[n] = k.dram(n, list(shape), dt, kind="ExternalInput")
    C.ident = k.sb("ident", [128, 128], F32)
    k.dma("sync", C.ident[:], C.cst['c_ident'][:])
    C.identb = k.sb("identb", [128, 128], BF16)
    k.op("vector", "tensor_copy", out=C.identb[:], in_=C.ident[:])
    C.psA = [k.ps("psA0", [128, 1024]), k.ps("psA1", [128, 1024])]
    C.psB = [k.ps("psB%d" % i, [128, 512]) for i in range(3)]
    C.psC = k.ps("psC", [128, 512])


def phase_mod(C, layers):
    k = C.k
    k.begin_phase()
    cT = k.sb("cT", [128, 8], F32)
    k.dma("sync", cT[:], colview(C.c_in, C.c_in.t), allow_slow_non_contiguous=True)
    cact = k.sb("cact", [128, 8], F32)
    k.op("scalar", "activation", out=cact[:], in_=cT[:], func=AF.Silu)
    wp = [k.sb("adaw%d" % j, [128, 3072], F32) for j in range(2)]
    brow = k.sb("adab", [1, 3072], F32)
    mrow = k.sb("modrow", [1, 3072], F32)
    banks = [C.psA[0][0:1, 0:512], C.psA[0][0:1, 512:1024], C.psA[1][0:1, 0:512],
             C.psA[1][0:1, 512:1024], C.psB[0][0:1, :], C.psB[1][0:1, :]]
    j = 0
    for i in layers:
        for hf in range(2):
            for kc in range(8):
                w = wp[j % 2]
                k.dma("sync" if j % 2 == 0 else "gpsimd", w[:],
                      C.P['ada_w'][i, kc * 128:(kc + 1) * 128, hf * 3072:(hf + 1) * 3072])
                j += 1
                for n in range(6):
                    k.op("tensor", "matmul", out=banks[n], lhsT=cact[:, kc:kc + 1],
                         rhs=w[:, n * 512:(n + 1) * 512], start=(kc == 0), stop=(kc == 7))
            k.dma("sync", brow[:], C.P['ada_b'][i:i + 1, hf * 3072:(hf + 1) * 3072])
            for n in range(6):
                k.op("vector", "tensor_tensor", out=mrow[:, n * 512:(n + 1) * 512], in0=banks[n],
                     in1=brow[:, n * 512:(n + 1) * 512], op=ALU.add)
            k.dma("sync", C.mod_d[i:i + 1, hf * 3072:(hf + 1) * 3072], mrow[:])
    k.end_phase()


def load_modcol(C, name, i, idx, plus1):
    k = C.k
    t = k.sb(name, [128, 8], F32)
    k.dma("sync", t[:], colview(C.mod_d, C.mod_d.t[i, idx * 1024:(idx + 1) * 1024]),
          allow_slow_non_contiguous=True)
    if plus1:
        k.op("vector", "tensor_scalar", out=t[:], in0=t[:], scalar1=1.0, scalar2=None, op0=ALU.add)
    return t


def load_bc(C, name, src_buf, ap1d, plus1=False, q="sync"):
    k = C.k
    n = ap1d.shape[0]
    t = k.sb(name, [128, n], F32)
    k.dma(q, t[:], bcast(src_buf, ap1d))
    if plus1:
        k.op("vector", "tensor_scalar", out=t[:], in0=t[:], scalar1=1.0, scalar2=None, op0=ALU.add)
    return t


def build_hT(C, xsrc, scp1, sh, hT, tok_tiles, xin, after_tile=None, h32=None):
    k = C.k
    for j, ti in enumerate(tok_tiles):
        xt = xin[j % len(xin)]
        k.dma("sync", xt[:], xsrc[ti * 128:(ti + 1) * 128, :])
        ps = C.psA[j % 2]
        for c in range(8):
            k.op("tensor", "transpose", out=ps[:, c * 128:(c + 1) * 128],
                 in_=xt[:, c * 128:(c + 1) * 128], identity=C.ident[:])
        for c in range(8):
            dst = hT[:, c, j * 128:(j + 1) * 128]
            if h32 is not None:
                k.op("scalar", "activation", out=h32[:, c, :], in_=ps[:, c * 128:(c + 1) * 128],
                     func=AF.Identity, bias=sh[:, c:c + 1], scale=scp1[:, c:c + 1])
                k.op("vector", "tensor_copy", out=dst, in_=h32[:, c, :])
            elif c % 2 == 0:
                k.op("vector", "tensor_scalar", out=dst, in0=ps[:, c * 128:(c + 1) * 128],
                     scalar1=scp1[:, c:c + 1], scalar2=sh[:, c:c + 1], op0=ALU.mult, op1=ALU.add)
            else:
                k.op("scalar", "activation", out=dst, in_=ps[:, c * 128:(c + 1) * 128],
                     func=AF.Identity, bias=sh[:, c:c + 1], scale=scp1[:, c:c + 1])
        if after_tile is not None:
            after_tile(j, ti)


def epilogue(C, ysrc, xold, mulG, lng, lnb, dst, tmp, addB=None):
    k = C.k
    z, st, mv = tmp
    if addB is not None:
        k.op("vector", "tensor_tensor", out=z[:], in0=ysrc, in1=addB[:], op=ALU.add)
        k.op("gpsimd", "tensor_tensor", out=z[:], in0=z[:], in1=mulG[:], op=ALU.mult)
    else:
        k.op("vector", "tensor_tensor", out=z[:], in0=ysrc, in1=mulG[:], op=ALU.mult)
    k.op("vector", "scalar_tensor_tensor", out=z[:], in0=xold[:], scalar=ALPHA_DN, in1=z[:],
         op0=ALU.mult, op1=ALU.add)
    k.op("vector", "bn_stats", out=st[:, 0, :], in_=z[:, 0:512])
    k.op("vector", "bn_stats", out=st[:, 1, :], in_=z[:, 512:1024])
    k.op("vector", "bn_aggr", out=mv[:, 0:2], in_=st[:])
    k.op("vector", "tensor_scalar", out=mv[:, 2:3], in0=mv[:, 1:2], scalar1=LN_EPS, scalar2=None,
         op0=ALU.add)
    k.op("scalar", "activation", out=mv[:, 3:4], in_=mv[:, 2:3], func=AF.Sqrt)
    k.op("vector", "reciprocal", out=mv[:, 4:5], in_=mv[:, 3:4])
    k.op("vector", "tensor_scalar", out=z[:], in0=z[:], scalar1=mv[:, 0:1], scalar2=mv[:, 4:5],
         op0=ALU.subtract, op1=ALU.mult)
    k.op("gpsimd", "tensor_tensor", out=z[:], in0=z[:], in1=lng[:], op=ALU.mult)
    k.op("vector", "tensor_tensor", out=z[:], in0=z[:], in1=lnb[:], op=ALU.add)
    k.dma("sync", dst, z[:])


def ep_tmp(C, n=2):
    k = C.k
    return [(k.sb("ep_z", [128, 1024], F32), k.sb("ep_st", [128, 2, 6], F32), k.sb("ep_mv", [128, 8], F32))
            for _ in range(n)]


def phase_moe(C, i, xsrc, xdst):
    k = C.k
    P = C.P
    rot = [0]
    import os
    for hf in range(int(os.environ.get('MOE_HALVES', '2'))):
        tiles = list(range(hf * 16, hf * 16 + 16))
        k.begin_phase()
        hT = k.sb("hT", [128, 8, 2048], BF16)
        gate = k.sb("gate", [128, 16, 32], F32)
        gateT = k.sb("gateT", [32, 2048], F32)
        yacc = k.sb("yacc", [128, 16, 1024], F32)
        b1T = k.sb("b1T", [128, 16, 32], F32)
        b1u7 = k.sb("b1u7", [128, 8, 32], F32)
        k.begin_phase()
        sc2 = load_modcol(C, "sc2", i, 4, True)
        sh2 = load_modcol(C, "sh2", i, 3, False)
        rw = k.sb("rw", [128, 8, 32], F32)
        k.dma("sync", rw[:], V(P['router_w'], P['router_w'].t[i].rearrange("(c p) e -> p c e", p=128)))
        rb = load_bc(C, "rb", P['router_b'], P['router_b'].t[i])
        b2 = k.sb("b2", [32, 1024], F32)
        k.dma("sync", b2[:], P['moe_b2'][i])
        b1raw = k.sb("b1raw", [32, 2048], F32)
        k.dma("sync", b1raw[:], P['moe_b1'][i])
        for c in range(0 if os.environ.get('MOE_NOB1') else 16):
            k.op("tensor", "transpose", out=C.psC[:, c * 32:(c + 1) * 32],
                 in_=b1raw[:, c * 128:(c + 1) * 128], identity=C.ident[0:32, 0:32])
        k.op("vector", "tensor_copy", out=b1T[:].rearrange("p c e -> p (c e)"), in_=C.psC[:, :])
        k.op("vector", "tensor_scalar", out=b1u7[:], in0=b1T[:, 8:16, :], scalar1=-1.0, scalar2=7.0,
             op0=ALU.mult, op1=ALU.add)
        xin = [k.sb("xin", [128, 1024], F32) for _ in range(2)]
        h32 = k.sb("h32", [128, 8, 128], F32)
        lg = k.sb("lg", [128, 32], F32)
        m8 = k.sb("m8", [128, 8], F32)
        negm = k.sb("negm", [128, 1], F32)
        msk = k.sb("msk", [128, 32], F32)
        ex = k.sb("ex", [128, 32], F32)
        ssum = k.sb("ssum", [128, 2], F32)

        def after_tile(j, ti):
            for c in range(8):
                k.op("tensor", "matmul", out=C.psC[:, 0:32], lhsT=h32[:, c, :], rhs=rw[:, c, :],
                     start=(c == 0), stop=(c == 7))
            k.op("vector", "tensor_tensor", out=lg[:], in0=C.psC[:, 0:32], in1=rb[:], op=ALU.add)
            k.op("vector", "max", out=m8[:], in_=lg[:])
            k.op("vector", "tensor_scalar", out=msk[:], in0=lg[:], scalar1=m8[:, 3:4], scalar2=None,
                 op0=ALU.is_ge)
            k.op("vector", "tensor_scalar", out=negm[:], in0=m8[:, 0:1], scalar1=-1.0, scalar2=None,
                 op0=ALU.mult)
            k.op("scalar", "activation", out=ex[:], in_=lg[:], func=AF.Exp, bias=negm[:, 0:1], scale=1.0)
            k.op("vector", "tensor_tensor", out=ex[:], in0=ex[:], in1=msk[:], op=ALU.mult)
            k.op("vector", "reduce_sum", out=ssum[:, 0:1], in_=ex[:], axis=AX.X)
            k.op("vector", "reciprocal", out=ssum[:, 1:2], in_=ssum[:, 0:1])
            k.op("vector", "tensor_scalar", out=gate[:, j, :], in0=ex[:], scalar1=ssum[:, 1:2],
                 scalar2=None, op0=ALU.mult)
            k.op("tensor", "transpose", out=C.psC[0:32, 128:256], in_=gate[:, j, :], identity=C.ident[:])
            k.op("scalar", "copy", out=gateT[:, j * 128:(j + 1) * 128], in_=C.psC[0:32, 128:256])
            for h in range(2):
                k.op("tensor", "matmul", out=C.psB[h][:, :], lhsT=gateT[:, j * 128:(j + 1) * 128],
                     rhs=b2[:, h * 512:(h + 1) * 512], start=True, stop=True)
                k.op("scalar", "copy", out=yacc[:, j, h * 512:(h + 1) * 512], in_=C.psB[h][:, :])

        build_hT(C, xsrc, sc2, sh2, hT, tiles, xin, after_tile=(None if os.environ.get('MOE_NOROUTER') else after_tile), h32=h32)
        k.end_phase()
        k.begin_phase()
        wbf = [dict(g=k.sb("w1g", [128, 8, 512], BF16), u=k.sb("w1u", [128, 8, 512], BF16),
                    d=k.sb("w2", [128, 4, 1024], BF16)) for _ in range(2)]
        stage = [k.sb("stg", [128, 2048], F32) for _ in range(3)]
        nT = 2
        tm = [dict(t1=k.sb("t1", [128, 256], F32), sg=k.sb("sg", [128, 256], F32),
                   r=k.sb("r", [128, 256], F32), nt3=k.sb("nt3", [128, 256], F32),
                   gs=k.sb("gs", [128, 256], F32), act=k.sb("act", [128, 256], BF16)) for _ in range(nT)]
        units = [(e, u) for e in range(NE) for u in range(2)][:int(os.environ.get('MOE_UNITS', '64'))]
        sidx = [0]

        def load_unit(n):
            e, u = units[n]
            slot = wbf[n % 2]
            w1 = P['moe_w1']
            w2 = P['moe_w2']
            for nm, col0 in (("g", u * 512), ("u", 1024 + u * 512)):
                for kh in range(2):
                    st = stage[sidx[0] % 3]
                    sidx[0] += 1
                    src = w1.t[i, e, kh * 512:(kh + 1) * 512, col0:col0 + 512].rearrange("(k p) f -> p k f", p=128)
                    k.dma("sync", V(st, st.t[:].rearrange("p (k f) -> p k f", k=4)), V(w1, src))
                    k.op("gpsimd", "tensor_copy", out=slot[nm][:, kh * 4:(kh + 1) * 4, :],
                         in_=V(st, st.t[:].rearrange("p (k f) -> p k f", k=4)))
            for rh in range(2):
                st = stage[sidx[0] % 3]
                sidx[0] += 1
                r0 = u * 512 + rh * 256
                src = w2.t[i, e, r0:r0 + 256, :].rearrange("(c p) d -> p c d", p=128)
                k.dma("sync", V(st, st.t[:].rearrange("p (c d) -> p c d", c=2)), V(w2, src))
                k.op("gpsimd", "tensor_copy", out=slot["d"][:, rh * 2:(rh + 1) * 2, :],
                     in_=V(st, st.t[:].rearrange("p (c d) -> p c d", c=2)))

        if os.environ.get('MOE_SKIP2'):
            units = []
        else:
            load_unit(0)
        for n, (e, u) in enumerate(units):
            if n + 1 < len(units):
                load_unit(n + 1)
            slot = wbf[n % 2]
            for tg in range(8):
                tsl = slice(tg * 256, (tg + 1) * 256)
                for fl in range(4):
                    fc = u * 4 + fl
                    pb = C.psB[rot[0] % 3]
                    T = tm[rot[0] % nT]
                    rot[0] += 1
                    for kc in range(8):
                        k.op("tensor", "matmul", out=pb[:, 0:256], lhsT=slot["g"][:, kc, fl * 128:(fl + 1) * 128],
                             rhs=hT[:, kc, tsl], start=(kc == 0), stop=(kc == 7))
                    for kc in range(8):
                        k.op("tensor", "matmul", out=pb[:, 256:512], lhsT=slot["u"][:, kc, fl * 128:(fl + 1) * 128],
                             rhs=hT[:, kc, tsl], start=(kc == 0), stop=(kc == 7))
                    k.op("vector", "tensor_scalar", out=T["t1"][:], in0=pb[:, 0:256],
                         scalar1=b1T[:, fc, e:e + 1], scalar2=7.0, op0=ALU.add, op1=ALU.min)
                    k.op("scalar", "activation", out=T["sg"][:], in_=T["t1"][:], func=AF.Sigmoid, scale=1.702)
                    k.op("scalar", "activation", out=T["r"][:], in_=pb[:, 256:512], func=AF.Relu,
                         bias=b1u7[:, fc, e:e + 1], scale=-1.0)
                    k.op("vector", "tensor_scalar", out=T["nt3"][:], in0=T["r"][:], scalar1=14.0, scalar2=8.0,
                         op0=ALU.min, op1=ALU.subtract)
                    k.op("gpsimd", "tensor_tensor", out=T["gs"][:], in0=T["t1"][:], in1=T["sg"][:], op=ALU.mult)
                    k.op("vector", "scalar_tensor_tensor", out=T["act"][:], in0=T["nt3"][:], scalar=-1.0,
                         in1=T["gs"][:], op0=ALU.mult, op1=ALU.mult)
                    for tt in range(2):
                        for dh in range(2):
                            k.op("tensor", "matmul", out=C.psA[tt][:, dh * 512:(dh + 1) * 512],
                                 lhsT=T["act"][:, tt * 128:(tt + 1) * 128],
                                 rhs=slot["d"][:, fl, dh * 512:(dh + 1) * 512], start=(fl == 0), stop=(fl == 3))
                for tt in range(2):
                    j = tg * 2 + tt
                    k.op("vector", "scalar_tensor_tensor", out=yacc[:, j, :], in0=C.psA[tt][:, :],
                         scalar=gate[:, j, e:e + 1], in1=yacc[:, j, :], op0=ALU.mult, op1=ALU.add)
        k.end_phase()
        k.begin_phase()
        G2 = load_bc(C, "G2", C.mod_d, C.mod_d.t[i, 5 * 1024:6 * 1024], plus1=True)
        lng = load_bc(C, "lng", P['ln2_g'], P['ln2_g'].t[i])
        lnb = load_bc(C, "lnb", P['ln2_b'], P['ln2_b'].t[i])
        xo = [k.sb("xo", [128, 1024], F32) for _ in range(2)]
        tmps = ep_tmp(C, 2)
        for j, ti in enumerate(tiles):
            k.dma("sync", xo[j % 2][:], xsrc[ti * 128:(ti + 1) * 128, :])
            epilogue(C, yacc[:, j, :], xo[j % 2], G2, lng, lnb, xdst[ti * 128:(ti + 1) * 128, :], tmps[j % 2])
        k.end_phase()
        k.end_phase()


def phase_pool(C, i, xsrc, xdst):
    k = C.k
    P = C.P
    k.begin_phase()
    hT = k.sb("hT", [128, 8, S], BF16)
    k.begin_phase()
    sc1 = load_modcol(C, "sc1", i, 1, True)
    sh1 = load_modcol(C, "sh1", i, 0, False)
    xin = [k.sb("xin", [128, 1024], F32) for _ in range(2)]
    build_hT(C, xsrc, sc1, sh1, hT, list(range(NT)), xin)
    k.end_phase()
    k.begin_phase()
    SA = k.sb("SA", [128, 16 + S], F32)
    SB = k.sb("SB", [128, 16 + S], F32)
    inv = k.sb("inv", [128, S], F32)
    k.op("vector", "memset", ap=SA[:, 0:16], constant=0.0)
    k.op("vector", "memset", ap=SB[:, 0:16], constant=0.0)
    for c in range(8):
        g = c // 2
        win = (2, 4, 8, 16)[g]
        if c % 2 == 0:
            k.dma("sync", inv[:], bcast(C.cst['c_poolinv'], C.cst['c_poolinv'].t[g]))
        k.op("scalar", "copy", out=SA[:, 16:], in_=hT[:, c, :])
        a, b = SA, SB
        s = 1
        while s < win:
            k.op("vector", "tensor_tensor", out=b[:, 16:], in0=a[:, 16:], in1=a[:, 16 - s:16 + S - s], op=ALU.add)
            a, b = b, a
            s *= 2
        k.op("gpsimd", "tensor_tensor", out=a[:, 16:], in0=a[:, 16:], in1=inv[:], op=ALU.mult)
        k.op("vector", "tensor_tensor", out=hT[:, c, :], in0=a[:, 16:], in1=hT[:, c, :], op=ALU.subtract)
    k.end_phase()
    k.begin_phase()
    wst = k.sb("pw_st", [128, 8, 256], F32)
    k.dma("sync", wst[:], V(P['pool_w'], P['pool_w'].t[0].rearrange("g (c p) d -> p (g c) d", p=128)))
    wb = k.sb("pw_bf", [128, 8, 256], BF16)
    k.op("vector", "tensor_copy", out=wb[:], in_=wst[:])
    pbias = load_bc(C, "pbias", P['pool_b'], P['pool_b'].t[0])
    pscale = load_bc(C, "pscale", P['pool_scale'], P['pool_scale'].t[0])
    G1 = load_bc(C, "G1", C.mod_d, C.mod_d.t[i, 2 * 1024:3 * 1024], plus1=True)
    k.op("vector", "tensor_tensor", out=G1[:], in0=G1[:], in1=pscale[:], op=ALU.mult)
    lng = load_bc(C, "lng", P['ln1_g'], P['ln1_g'].t[i])
    lnb = load_bc(C, "lnb", P['ln1_b'], P['ln1_b'].t[i])
    xo = [k.sb("xo", [128, 1024], F32) for _ in range(2)]
    tmps = ep_tmp(C, 2)
    for ti in range(NT):
        ps = C.psA[ti % 2]
        for g in range(4):
            for kc in range(2):
                k.op("tensor", "matmul", out=ps[:, g * 256:(g + 1) * 256],
                     lhsT=hT[:, 2 * g + kc, ti * 128:(ti + 1) * 128], rhs=wb[:, 2 * g + kc, :],
                     start=(kc == 0), stop=(kc == 1))
        k.dma("sync", xo[ti % 2][:], xsrc[ti * 128:(ti + 1) * 128, :])
        epilogue(C, ps[:, :], xo[ti % 2], G1, lng, lnb, xdst[ti * 128:(ti + 1) * 128, :], tmps[ti % 2], addB=pbias)
    k.end_phase()
    k.end_phase()


def load_cast(C, dst, src_buf, src_ap, stage, q="sync", eng="gpsimd"):
    k = C.k
    k.dma(q, stage, V(src_buf, src_ap))
    k.op(eng, "tensor_copy", out=dst, in_=stage)


def phase_conv(C, i, xsrc, xdst):
    k = C.k
    P = C.P
    k.begin_phase()
    vT = k.sb("vT", [128, 8, S], BF16)
    k.begin_phase()
    hT = k.sb("hT", [128, 8, S], BF16)
    k.begin_phase()
    sc1 = load_modcol(C, "sc1", i, 1, True)
    sh1 = load_modcol(C, "sh1", i, 0, False)
    xin = [k.sb("xin", [128, 1024], F32) for _ in range(2)]
    build_hT(C, xsrc, sc1, sh1, hT, list(range(NT)), xin)
    k.end_phase()
    k.begin_phase()
    bpw = k.sb("bpw", [128, 16], F32)
    k.dma("sync", bpw[:], colview(P['conv_b_pw1'], P['conv_b_pw1'].t[0]), allow_slow_non_contiguous=True)
    bdw = k.sb("bdw", [128, 8], F32)
    k.dma("sync", bdw[:], colview(P['conv_b_dw'], P['conv_b_dw'].t[0]), allow_slow_non_contiguous=True)
    wraw = k.sb("wdwraw", [31, 1024], F32)
    k.dma("sync", wraw[:], P['conv_w_dw'][0])
    wdw = k.sb("wdw", [128, 8, 31], F32)
    for c in range(8):
        k.op("tensor", "transpose", out=C.psC[:, c * 31:(c + 1) * 31], in_=wraw[:, c * 128:(c + 1) * 128],
             identity=C.ident[0:31, 0:31])
    k.op("vector", "tensor_copy", out=wdw[:].rearrange("p c k -> p (c k)"), in_=C.psC[:, 0:248])
    U = k.sb("U", [128, 30 + S], F32)
    acc = k.sb("acc", [128, S], F32)
    k.op("vector", "memset", ap=U[:, 0:30], constant=0.0)
    wst = k.sb("wst", [128, 8, 256], F32)
    wbf = [k.sb("wpw1", [128, 8, 256], BF16) for _ in range(2)]
    sg = [k.sb("sg", [128, 512], F32) for _ in range(2)]
    W1 = P['conv_w_pw1']
    for dc in range(8):
        w = wbf[dc % 2]
        for half, col0 in ((0, dc * 128), (1, 1024 + dc * 128)):
            load_cast(C, w[:, :, half * 128:(half + 1) * 128], W1,
                      W1.t[0, :, col0:col0 + 128].rearrange("(k p) f -> p k f", p=128),
                      wst[:, :, half * 128:(half + 1) * 128])
        for tg in range(8):
            tsl = slice(tg * 512, (tg + 1) * 512)
            pa, pg = C.psB[0], C.psB[1]
            for kc in range(8):
                k.op("tensor", "matmul", out=pa[:, :], lhsT=w[:, kc, 0:128], rhs=hT[:, kc, tsl],
                     start=(kc == 0), stop=(kc == 7))
            for kc in range(8):
                k.op("tensor", "matmul", out=pg[:, :], lhsT=w[:, kc, 128:256], rhs=hT[:, kc, tsl],
                     start=(kc == 0), stop=(kc == 7))
            s_ = sg[tg % 2]
            k.op("scalar", "activation", out=s_[:], in_=pg[:, :], func=AF.Sigmoid,
                 bias=bpw[:, 8 + dc:9 + dc], scale=1.0)
            k.op("vector", "scalar_tensor_tensor", out=U[:, 30 + tg * 512:30 + (tg + 1) * 512], in0=pa[:, :],
                 scalar=bpw[:, dc:dc + 1], in1=s_[:], op0=ALU.add, op1=ALU.mult)
        k.op("vector", "tensor_scalar", out=acc[:], in0=U[:, 0:S], scalar1=wdw[:, dc, 0:1], scalar2=None,
             op0=ALU.mult)
        for t in range(1, 31):
            k.op("vector", "scalar_tensor_tensor", out=acc[:], in0=U[:, t:t + S], scalar=wdw[:, dc, t:t + 1],
                 in1=acc[:], op0=ALU.mult, op1=ALU.add)
        k.op("scalar", "activation", out=vT[:, dc, :], in_=acc[:], func=AF.Identity, bias=bdw[:, dc:dc + 1],
             scale=1.0)
    k.end_phase()
    k.end_phase()
    k.begin_phase()
    ones = k.sb("ones", [128, 128], BF16)
    k.op("vector", "memset", ap=ones[:], constant=1.0)
    lgc = k.sb("clng", [128, 8], F32)
    k.dma("sync", lgc[:], colview(P['conv_ln_g'], P['conv_ln_g'].t[0]), allow_slow_non_contiguous=True)
    lbc = k.sb("clnb", [128, 8], F32)
    k.dma("sync", lbc[:], colview(P['conv_ln_b'], P['conv_ln_b'].t[0]), allow_slow_non_contiguous=True)
    sq = [k.sb("sq", [128, 512], BF16) for _ in range(2)]
    mean = k.sb("mean", [128, 512], F32)
    var = k.sb("var", [128, 512], F32)
    rstd = k.sb("rstd", [128, 512], F32)
    tt = [k.sb("tt", [128, 512], F32) for _ in range(2)]
    for tg in range(8):
        tsl = slice(tg * 512, (tg + 1) * 512)
        p1, p2 = C.psB[0], C.psB[1]
        for dc in range(8):
            k.op("tensor", "matmul", out=p1[:, :], lhsT=ones[:], rhs=vT[:, dc, tsl], start=(dc == 0), stop=(dc == 7))
        for dc in range(8):
            s_ = sq[dc % 2]
            k.op("scalar", "activation", out=s_[:], in_=vT[:, dc, tsl], func=AF.Square)
            k.op("tensor", "matmul", out=p2[:, :], lhsT=ones[:], rhs=s_[:], start=(dc == 0), stop=(dc == 7))
        k.op("vector", "tensor_scalar", out=mean[:], in0=p1[:, :], scalar1=1.0 / D, scalar2=None, op0=ALU.mult)
        k.op("vector", "tensor_tensor", out=var[:], in0=mean[:], in1=mean[:], op=ALU.mult)
        k.op("vector", "scalar_tensor_tensor", out=var[:], in0=p2[:, :], scalar=1.0 / D, in1=var[:],
             op0=ALU.mult, op1=ALU.subtract)
        k.op("vector", "tensor_scalar", out=var[:], in0=var[:], scalar1=LN_EPS, scalar2=None, op0=ALU.add)
        k.op("scalar", "activation", out=var[:], in_=var[:], func=AF.Sqrt)
        k.op("vector", "reciprocal", out=rstd[:], in_=var[:])
        for dc in range(8):
            t_ = tt[dc % 2]
            k.op("vector", "tensor_tensor", out=t_[:], in0=vT[:, dc, tsl], in1=mean[:], op=ALU.subtract)
            k.op("gpsimd", "tensor_tensor", out=t_[:], in0=t_[:], in1=rstd[:], op=ALU.mult)
            k.op("scalar", "activation", out=vT[:, dc, tsl], in_=t_[:], func=AF.Silu, bias=lbc[:, dc:dc + 1],
                 scale=lgc[:, dc:dc + 1])
    k.end_phase()
    k.begin_phase()
    wst2 = k.sb("wst2", [128, 8, 1024], F32)
    w2 = k.sb("wpw2", [128, 8, 1024], BF16)
    W2 = P['conv_w_pw2']
    load_cast(C, w2[:], W2, W2.t[0].rearrange("(k p) f -> p k f", p=128), wst2[:])
    bb = load_bc(C, "bpw2", P['conv_b_pw2'], P['conv_b_pw2'].t[0])
    G1 = load_bc(C, "G1", C.mod_d, C.mod_d.t[i, 2 * 1024:3 * 1024], plus1=True)
    lng = load_bc(C, "lng", P['ln1_g'], P['ln1_g'].t[i])
    lnb = load_bc(C, "lnb", P['ln1_b'], P['ln1_b'].t[i])
    xo = [k.sb("xo", [128, 1024], F32) for _ in range(2)]
    tmps = ep_tmp(C, 2)
    for ti in range(NT):
        ps = C.psA[ti % 2]
        for dh in range(2):
            for kc in range(8):
                k.op("tensor", "matmul", out=ps[:, dh * 512:(dh + 1) * 512], lhsT=vT[:, kc, ti * 128:(ti + 1) * 128],
                     rhs=w2[:, kc, dh * 512:(dh + 1) * 512], start=(kc == 0), stop=(kc == 7))
        k.dma("sync", xo[ti % 2][:], xsrc[ti * 128:(ti + 1) * 128, :])
        epilogue(C, ps[:, :], xo[ti % 2], G1, lng, lnb, xdst[ti * 128:(ti + 1) * 128, :], tmps[ti % 2], addB=bb)
    k.end_phase()
    k.end_phase()


def rope_tables(C, dhalf, inv_name):
    k = C.k
    posi = k.sb("posi", [128, S], I32)
    k.dma("sync", posi[:], bcast(C.pos_in, C.pos_in.t))
    ang = k.sb("ang", [128, S], F32)
    k.op("vector", "tensor_copy", out=ang[:], in_=posi[:])
    inv = k.sb("ropeinv", [128, 1], F32)
    k.dma("sync", inv[:], C.cst[inv_name][:])
    k.op("vector", "tensor_scalar", out=ang[:], in0=ang[:], scalar1=inv[:, 0:1], scalar2=None, op0=ALU.mult)
    return ang, posi


def rope_finish(C, ang, cosT, sinT, tmp, tmpi):
    k = C.k
    TWO_PI = 2.0 * np.pi
    msk = tmpi.bitcast(F32) if False else None
    for dst, shift in ((sinT, 0.0), (cosT, np.pi / 2.0)):
        if shift != 0.0:
            k.op("vector", "tensor_scalar", out=ang[:], in0=ang[:], scalar1=shift, scalar2=None, op0=ALU.add)
        k.op("vector", "tensor_scalar", out=tmpi[:], in0=ang[:], scalar1=1.0 / TWO_PI, scalar2=None, op0=ALU.mult)
        k.op("vector", "tensor_copy", out=tmp[:], in_=tmpi[:])
        k.op("vector", "scalar_tensor_tensor", out=tmp[:], in0=tmp[:], scalar=-TWO_PI, in1=ang[:], op0=ALU.mult, op1=ALU.add)
        k.op("vector", "tensor_scalar", out=tmpi[:], in0=tmp[:], scalar1=np.pi, scalar2=None, op0=ALU.is_gt)
        k.op("vector", "tensor_copy", out=dst[:], in_=tmpi[:])
        k.op("vector", "scalar_tensor_tensor", out=tmp[:], in0=dst[:], scalar=-TWO_PI, in1=tmp[:], op0=ALU.mult, op1=ALU.add)
        k.op("vector", "tensor_scalar", out=tmp[:], in0=tmp[:], scalar1=-np.pi, scalar2=np.pi, op0=ALU.max, op1=ALU.min)
        k.op("scalar", "activation", out=dst[:], in_=tmp[:], func=AF.Sin)


def phase_ret(C, i, xsrc, xdst):
    k = C.k
    P = C.P
    Gd = k.dram("ret_gated", [S, 2048], F32)
    Win = P['ret_w_in']
    k.begin_phase()
    hT = k.sb("hT", [128, 8, S], BF16)
    cosT = k.sb("cosT", [128, S], BF16)
    sinT = k.sb("sinT", [128, S], BF16)
    k.begin_phase()
    ang, posi = rope_tables(C, 128, 'c_ropeinv_ret')
    tmp = k.sb("angtmp", [128, S], F32)
    rope_finish(C, ang, cosT, sinT, tmp, posi)
    k.end_phase()
    k.begin_phase()
    sc1 = load_modcol(C, "sc1", i, 1, True)
    sh1 = load_modcol(C, "sh1", i, 0, False)
    xin = [k.sb("xin", [128, 1024], F32) for _ in range(2)]
    build_hT(C, xsrc, sc1, sh1, hT, list(range(NT)), xin)
    k.end_phase()
    k.begin_phase()
    qT = k.sb("qT", [128, 2, S], BF16)
    kT = k.sb("kT", [128, 2, S], BF16)
    v = k.sb("v", [128, NT, 512], BF16)
    wq = k.sb("wq", [128, 8, 512], BF16)
    wv = k.sb("wv", [128, 8, 512], BF16)
    wg = k.sb("wg", [128, 8, 512], BF16)
    wst = k.sb("wst", [128, 8, 256], F32)
    D0 = k.sb("D0", [128, 128], F32)
    D1 = k.sb("D1", [128, 128], F32)
    gng = k.sb("gng", [128, 512], F32)
    gnb = k.sb("gnb", [128, 512], F32)
    rt = [k.sb("rt%d" % j, [128, 512], F32) for j in range(4)]
    pT = [k.sb("pT", [128, 128], BF16) for _ in range(3)]
    st = k.sb("gst", [128, 6], F32)
    mv = k.sb("gmv", [128, 8], F32)
    on = [k.sb("on", [128, 512], F32) for _ in range(2)]
    sil = [k.sb("sil", [128, 512], F32) for _ in range(2)]
    rot = 0
    for hd in range(4):
        gam = 1.0 - 2.0 ** (-5.0 - hd)
        for half, col0 in ((0, hd * 256), (1, 1024 + hd * 256)):
            load_cast(C, wq[:, :, half * 256:(half + 1) * 256], Win,
                      Win.t[0, :, col0:col0 + 256].rearrange("(k p) f -> p k f", p=128), wst[:])
        for half in range(2):
            c0 = 2048 + hd * 512 + half * 256
            load_cast(C, wv[:, :, half * 256:(half + 1) * 256], Win,
                      Win.t[0, :, c0:c0 + 256].rearrange("(k p) f -> p k f", p=128), wst[:])
            c0 = 4096 + hd * 512 + half * 256
            load_cast(C, wg[:, :, half * 256:(half + 1) * 256], Win,
                      Win.t[0, :, c0:c0 + 256].rearrange("(k p) f -> p k f", p=128), wst[:])
        k.dma("sync", D0[:], C.cst['c_retD'][hd, 0])
        k.dma("sync", D1[:], C.cst['c_retD'][hd, 1])
        k.dma("sync", gng[:], bcast(P['ret_gn_g'], P['ret_gn_g'].t[0, hd * 512:(hd + 1) * 512]))
        k.dma("sync", gnb[:], bcast(P['ret_gn_b'], P['ret_gn_b'].t[0, hd * 512:(hd + 1) * 512]))
        for dstT, base in ((qT, 0), (kT, 256)):
            for tg in range(8):
                tsl = slice(tg * 512, (tg + 1) * 512)
                pa, pb = C.psB[0], C.psB[1]
                for kc in range(8):
                    k.op("tensor", "matmul", out=pa[:, :], lhsT=wq[:, kc, base:base + 128], rhs=hT[:, kc, tsl],
                         start=(kc == 0), stop=(kc == 7))
                for kc in range(8):
                    k.op("tensor", "matmul", out=pb[:, :], lhsT=wq[:, kc, base + 128:base + 256], rhs=hT[:, kc, tsl],
                         start=(kc == 0), stop=(kc == 7))
                k.op("vector", "tensor_tensor", out=rt[0][:], in0=pa[:, :], in1=cosT[:, tsl], op=ALU.mult)
                k.op("vector", "tensor_tensor", out=rt[1][:], in0=pb[:, :], in1=sinT[:, tsl], op=ALU.mult)
                k.op("vector", "tensor_tensor", out=rt[2][:], in0=pa[:, :], in1=sinT[:, tsl], op=ALU.mult)
                k.op("vector", "tensor_tensor", out=rt[3][:], in0=pb[:, :], in1=cosT[:, tsl], op=ALU.mult)
                k.op("gpsimd", "tensor_tensor", out=dstT[:, 0, tsl], in0=rt[0][:], in1=rt[1][:], op=ALU.subtract)
                k.op("gpsimd", "tensor_tensor", out=dstT[:, 1, tsl], in0=rt[2][:], in1=rt[3][:], op=ALU.add)
        for ti in range(NT):
            pv = C.psB[ti % 2]
            for kc in range(8):
                k.op("tensor", "matmul", out=pv[:, :], lhsT=hT[:, kc, ti * 128:(ti + 1) * 128], rhs=wv[:, kc, :],
                     start=(kc == 0), stop=(kc == 7))
            k.op("scalar", "copy", out=v[:, ti, :], in_=pv[:, :])
        for qt in range(NT):
            po = C.psA[qt % 2]
            for kt in range(qt + 1):
                pS = C.psB[rot % 3]
                pt = pT[rot % 3]
                rot += 1
                for c in range(2):
                    k.op("tensor", "matmul", out=pS[:, 0:128], lhsT=kT[:, c, kt * 128:(kt + 1) * 128],
                         rhs=qT[:, c, qt * 128:(qt + 1) * 128], start=(c == 0), stop=(c == 1))
                if kt == qt:
                    k.op("vector", "tensor_tensor", out=pt[:], in0=pS[:, 0:128], in1=D0[:], op=ALU.mult)
                else:
                    f = gam ** (128.0 * (qt - kt - 1))
                    k.op("vector", "scalar_tensor_tensor", out=pt[:], in0=pS[:, 0:128], scalar=float(f), in1=D1[:],
                         op0=ALU.mult, op1=ALU.mult)
                k.op("tensor", "matmul", out=po[:, 0:512], lhsT=pt[:], rhs=v[:, kt, :], start=(kt == 0), stop=(kt == qt))
            pg = C.psC
            for kc in range(8):
                k.op("tensor", "matmul", out=pg[:, :], lhsT=hT[:, kc, qt * 128:(qt + 1) * 128], rhs=wg[:, kc, :],
                     start=(kc == 0), stop=(kc == 7))
            s_ = sil[qt % 2]
            k.op("scalar", "activation", out=s_[:], in_=pg[:, :], func=AF.Silu)
            o_ = on[qt % 2]
            k.op("vector", "bn_stats", out=st[:], in_=po[:, 0:512])
            k.op("vector", "bn_aggr", out=mv[:, 0:2], in_=st[:])
            k.op("vector", "tensor_scalar", out=mv[:, 2:3], in0=mv[:, 1:2], scalar1=LN_EPS, scalar2=None, op0=ALU.add)
            k.op("scalar", "activation", out=mv[:, 3:4], in_=mv[:, 2:3], func=AF.Sqrt)
            k.op("vector", "reciprocal", out=mv[:, 4:5], in_=mv[:, 3:4])
            k.op("vector", "tensor_scalar", out=o_[:], in0=po[:, 0:512], scalar1=mv[:, 0:1], scalar2=mv[:, 4:5],
                 op0=ALU.subtract, op1=ALU.mult)
            k.op("gpsimd", "tensor_tensor", out=o_[:], in0=o_[:], in1=gng[:], op=ALU.mult)
            k.op("gpsimd", "tensor_tensor", out=o_[:], in0=o_[:], in1=gnb[:], op=ALU.add)
            k.op("vector", "tensor_tensor", out=o_[:], in0=o_[:], in1=s_[:], op=ALU.mult)
            k.dma("sync", Gd[qt * 128:(qt + 1) * 128, hd * 512:(hd + 1) * 512], o_[:])
    k.end_phase()
    k.end_phase()
    k.begin_phase()
    wo = k.sb("wo", [128, 16, 1024], BF16)
    wst2 = k.sb("wst2", [128, 4, 1024], F32)
    Wo = P['ret_w_out']
    for q4 in range(4):
        load_cast(C, wo[:, q4 * 4:(q4 + 1) * 4, :], Wo,
                  Wo.t[0, q4 * 512:(q4 + 1) * 512, :].rearrange("(k p) f -> p k f", p=128), wst2[:])
    G1 = load_bc(C, "G1", C.mod_d, C.mod_d.t[i, 2 * 1024:3 * 1024], plus1=True)
    lng = load_bc(C, "lng", P['ln1_g'], P['ln1_g'].t[i])
    lnb = load_bc(C, "lnb", P['ln1_b'], P['ln1_b'].t[i])
    xo = [k.sb("xo", [128, 1024], F32) for _ in range(2)]
    gin = [k.sb("gin", [128, 2048], F32) for _ in range(2)]
    gT = [k.sb("gT", [128, 16, 128], BF16) for _ in range(2)]
    tmps = ep_tmp(C, 2)
    for ti in range(NT):
        g_ = gin[ti % 2]
        k.dma("sync", g_[:], Gd[ti * 128:(ti + 1) * 128, :])
        for g4 in range(4):
            bank = C.psB[g4 % 2]
            for c in range(4):
                cidx = g4 * 4 + c
                k.op("tensor", "transpose", out=bank[:, c * 128:(c + 1) * 128],
                     in_=g_[:, cidx * 128:(cidx + 1) * 128], identity=C.ident[:])
            dstv = gT[ti % 2][:, g4 * 4:(g4 + 1) * 4, :].rearrange("p c t -> p (c t)")
            if g4 % 2 == 0:
                k.op("vector", "tensor_copy", out=dstv, in_=bank[:, :])
            else:
                k.op("scalar", "copy", out=dstv, in_=bank[:, :])
        ps = C.psA[ti % 2]
        for dh in range(2):
            for kc in range(16):
                k.op("tensor", "matmul", out=ps[:, dh * 512:(dh + 1) * 512], lhsT=gT[ti % 2][:, kc, :],
                     rhs=wo[:, kc, dh * 512:(dh + 1) * 512], start=(kc == 0), stop=(kc == 15))
        k.dma("sync", xo[ti % 2][:], xsrc[ti * 128:(ti + 1) * 128, :])
        epilogue(C, ps[:, :], xo[ti % 2], G1, lng, lnb, xdst[ti * 128:(ti + 1) * 128, :], tmps[ti % 2])
    k.end_phase()


NSA_BIG = 30000.0


def phase_nsa(C, i, xsrc, xdst):
    k = C.k
    P = C.P
    W = P['nsa_w_in']
    Qd = k.dram("nsa_q", [8, 128, S], BF16)
    Od = k.dram("nsa_o", [S, D], F32)
    k.begin_phase()
    kT = {nm: [k.sb("kT_%s%d" % (nm, g), [128, S], BF16) for g in range(2)] for nm in ("kc", "ks", "kw", "vc")}
    vx = {nm: k.sb("vx_" + nm, [128, NT, 2, 66], BF16) for nm in ("vs", "vw")}
    gates = k.sb("gates", [128, NT, 48], F32)
    for nm in ("vs", "vw"):
        k.op("vector", "memset", ap=vx[nm][:], constant=1.0)
    k.begin_phase()
    cosT = k.sb("cosT", [128, S], BF16)
    sinT = k.sb("sinT", [128, S], BF16)
    k.begin_phase()
    ang, posi = rope_tables(C, 32, 'c_ropeinv_nsa')
    tmp = k.sb("angtmp", [128, S], F32)
    rope_finish(C, ang, cosT, sinT, tmp, posi)
    sgn = k.sb("sgn", [128, 1], F32)
    k.dma("sync", sgn[:], C.cst['c_ropesgn'][:])
    k.op("vector", "tensor_scalar", out=sinT[:], in0=sinT[:], scalar1=sgn[:, 0:1], scalar2=None, op0=ALU.mult)
    k.end_phase()
    k.begin_phase()
    hT = k.sb("hT", [128, 8, S], BF16)
    k.begin_phase()
    sc1 = load_modcol(C, "sc1", i, 1, True)
    sh1 = load_modcol(C, "sh1", i, 0, False)
    xin = [k.sb("xin", [128, 1024], F32) for _ in range(2)]
    build_hT(C, xsrc, sc1, sh1, hT, list(range(NT)), xin)
    k.end_phase()
    k.begin_phase()
    stn = k.sb("stn", [128, 8, 128], F32)
    sts = k.sb("sts", [128, 8, 128], F32)
    wn = [k.sb("wn", [128, 8, 128], BF16) for _ in range(2)]
    ws = [k.sb("ws", [128, 8, 128], BF16) for _ in range(2)]
    t0 = [k.sb("t0", [128, 512], F32) for _ in range(2)]
    t1 = [k.sb("t1", [128, 512], F32) for _ in range(2)]
    qst = [k.sb("qst", [128, 512], BF16) for _ in range(2)]
    chunks = [("q", c, (c * 128, c * 128 + 64), True, 0.125) for c in range(8)]
    for nm, base in (("kc", 1024), ("ks", 1280), ("kw", 1536)):
        for g in range(2):
            chunks.append((nm, g, (base + g * 64, base + g * 64), True, 1.0))
    for g in range(2):
        chunks.append(("vc", g, (1152 + g * 64, 1152 + g * 64), False, 1.0))

    def wsrc(c0, n):
        return V(W, W.t[0, :, c0:c0 + n].rearrange("(k p) f -> p k f", p=128))

    import os
    for ci, (nm, idx, blocks, rope, scale) in enumerate(chunks[:int(os.environ.get('NSA_NCHUNK', '99'))]):
        wn_, ws_ = wn[ci % 2], ws[ci % 2]
        for b, c0 in enumerate(blocks):
            k.dma("sync", stn[:, :, b * 64:(b + 1) * 64], wsrc(c0, 64))
            if rope:
                k.dma("sync", sts[:, :, b * 64:b * 64 + 32], wsrc(c0 + 32, 32))
                k.dma("sync", sts[:, :, b * 64 + 32:b * 64 + 64], wsrc(c0, 32))
        k.op("gpsimd", "tensor_copy", out=wn_[:], in_=stn[:])
        if rope:
            k.op("gpsimd", "tensor_copy", out=ws_[:], in_=sts[:])
        for tg in range(8):
            tsl = slice(tg * 512, (tg + 1) * 512)
            pa, pb = C.psB[0], C.psB[1]
            for kc in range(8):
                k.op("tensor", "matmul", out=pa[:, :], lhsT=wn_[:, kc, :], rhs=hT[:, kc, tsl], start=(kc == 0), stop=(kc == 7))
            if rope:
                for kc in range(8):
                    k.op("tensor", "matmul", out=pb[:, :], lhsT=ws_[:, kc, :], rhs=hT[:, kc, tsl], start=(kc == 0), stop=(kc == 7))
                a_, b_ = t0[tg % 2], t1[tg % 2]
                k.op("vector", "scalar_tensor_tensor", out=a_[:], in0=pa[:, :], scalar=scale, in1=cosT[:, tsl], op0=ALU.mult, op1=ALU.mult)
                k.op("vector", "scalar_tensor_tensor", out=b_[:], in0=pb[:, :], scalar=scale, in1=sinT[:, tsl], op0=ALU.mult, op1=ALU.mult)
                if nm == "q":
                    qs = qst[tg % 2]
                    k.op("gpsimd", "tensor_tensor", out=qs[:], in0=a_[:], in1=b_[:], op=ALU.add)
                    k.dma("sync", Qd[idx, :, tsl], qs[:])
                else:
                    k.op("gpsimd", "tensor_tensor", out=kT[nm][idx][:, tsl], in0=a_[:], in1=b_[:], op=ALU.add)
            else:
                k.op("scalar", "copy", out=kT[nm][idx][:, tsl], in_=pa[:, :])
    k.end_phase()
    k.begin_phase()
    stv = k.sb("stv", [128, 8, 304], F32)
    wv = k.sb("wvg", [128, 8, 304], BF16)
    k.dma("sync", stv[:, :, 0:128], wsrc(1408, 128))
    k.dma("sync", stv[:, :, 128:256], wsrc(1664, 128))
    k.dma("sync", stv[:, :, 256:304], wsrc(1792, 48))
    k.op("gpsimd", "tensor_copy", out=wv[:], in_=stv[:])
    gb = load_bc(C, "gateb", P['nsa_gate_b'], P['nsa_gate_b'].t[0])
    gl = [k.sb("gl", [128, 48], F32) for _ in range(2)]
    pvs = [k.sb("pvs", [128, 304], F32) for _ in range(2)]
    for ti in range(0 if os.environ.get('NSA_SKIP_TOK') else NT):
        pv = C.psB[ti % 2]
        for kc in range(8):
            k.op("tensor", "matmul", out=pv[:, 0:304], lhsT=hT[:, kc, ti * 128:(ti + 1) * 128], rhs=wv[:, kc, :], start=(kc == 0), stop=(kc == 7))
        ev = pvs[ti % 2]
        k.op("scalar", "copy", out=ev[:], in_=pv[:, 0:304])
        k.op("vector", "tensor_copy", out=vx["vs"][:, ti, 0, 0:64], in_=ev[:, 0:64])
        k.op("vector", "tensor_copy", out=vx["vs"][:, ti, 1, 0:64], in_=ev[:, 64:128])
        k.op("gpsimd", "tensor_copy", out=vx["vw"][:, ti, 0, 0:64], in_=ev[:, 128:192])
        k.op("gpsimd", "tensor_copy", out=vx["vw"][:, ti, 1, 0:64], in_=ev[:, 192:256])
        k.op("vector", "tensor_tensor", out=gl[ti % 2][:], in0=ev[:, 256:304], in1=gb[:], op=ALU.add)
        k.op("scalar", "activation", out=gates[:, ti, :], in_=gl[ti % 2][:], func=AF.Sigmoid)
    k.end_phase()
    k.end_phase()
    k.end_phase()
    k.begin_phase()
    kcmpT = k.sb("kcmpT", [128, 2, 256], BF16)
    vcx = k.sb("vcx", [128, 2, 2, 130], BF16)
    k.op("vector", "memset", ap=vcx[:], constant=0.0)
    k.op("vector", "memset", ap=kcmpT[:], constant=0.0)
    k.begin_phase()
    ovst = k.sb("ovst", [128, 2, 64], F32)
    k.dma("sync", ovst[:], V(C.cst['c_ovl'], C.cst['c_ovl'].t.rearrange("t p j -> p t j")))
    onst = k.sb("onst", [128, 2, 1], F32)
    k.dma("sync", onst[:], C.cst['c_cmpones'][:])
    for g in range(2):
        k.op("vector", "tensor_copy", out=vcx[:, :, g, 65:129], in_=ovst[:])
        k.op("vector", "tensor_copy", out=vcx[:, :, g, 64:65], in_=onst[:])
    w1st = k.sb("w1st", [64, 32, 256], F32)
    w1 = k.sb("w1bf", [64, 32, 256], BF16)
    w2st = k.sb("w2st", [128, 2, 64], F32)
    w2 = k.sb("w2bf", [128, 2, 128], BF16)
    posT = k.sb("posT", [64, 32], F32)
    blk = [k.sb("blk", [64, 256], BF16) for _ in range(3)]
    hid = k.sb("hid", [128, 2, 256], BF16)
    ht = [k.sb("ht%d" % j, [128, 256], F32) for j in range(3)]
    k.op("vector", "memset", ap=hid[:], constant=0.0)
    import os
    for which, srcname, w1n, w2n, pn in ([] if os.environ.get('NSA_SKIP_CMP') else [("k", "kc", 'nsa_cmp_k_w1', 'nsa_cmp_k_w2', 'nsa_cmp_pos_k'),
                                         ("v", "vc", 'nsa_cmp_v_w1', 'nsa_cmp_v_w2', 'nsa_cmp_pos_v')]):
        k.dma("sync", w1st[:], V(P[w1n], P[w1n].t[0].rearrange("(l d) h -> d l h", d=64)))
        k.op("gpsimd", "tensor_copy", out=w1[:], in_=w1st[:])
        k.dma("sync", w2st[:], V(P[w2n], P[w2n].t[0].rearrange("(c p) d -> p c d", p=128)))
        k.op("vector", "tensor_copy", out=w2[:, :, 0:64], in_=w2st[:])
        k.op("vector", "tensor_copy", out=w2[:, :, 64:128], in_=w2st[:])
        k.dma("sync", posT[:], V(P[pn], P[pn].t[0].rearrange("l d -> d l")), allow_slow_non_contiguous=True)
        for g in range(2):
            src = kT[srcname][g]
            for hc in range(2):
                ph = C.psB[hc]
                for l in range(32):
                    b_ = blk[l % 3]
                    k.op("vector", "tensor_scalar", out=b_[:, 0:255], in0=src[0:64, l:l + 16 * 254 + 1:16],
                         scalar1=posT[:, l:l + 1], scalar2=None, op0=ALU.add)
                    k.op("tensor", "matmul", out=ph[:, 0:255], lhsT=w1[:, l, hc * 128:(hc + 1) * 128], rhs=b_[:, 0:255],
                         start=(l == 0), stop=(l == 31))
                x_, u_, s_ = ht
                k.op("scalar", "copy", out=x_[:, 0:255], in_=ph[:, 0:255])
                k.op("vector", "tensor_tensor", out=u_[:, 0:255], in0=x_[:, 0:255], in1=x_[:, 0:255], op=ALU.mult)
                k.op("vector", "tensor_scalar", out=u_[:, 0:255], in0=u_[:, 0:255], scalar1=0.044715, scalar2=1.0, op0=ALU.mult, op1=ALU.add)
                k.op("vector", "tensor_tensor", out=u_[:, 0:255], in0=u_[:, 0:255], in1=x_[:, 0:255], op=ALU.mult)
                k.op("scalar", "activation", out=s_[:, 0:255], in_=u_[:, 0:255], func=AF.Sigmoid, scale=1.5957691216057308)
                k.op("vector", "tensor_tensor", out=hid[:, hc, 0:255], in0=x_[:, 0:255], in1=s_[:, 0:255], op=ALU.mult)
            if which == "k":
                pk = C.psC
                for hc in range(2):
                    k.op("tensor", "matmul", out=pk[:, 0:255], lhsT=w2[:, hc, :], rhs=hid[:, hc, 0:255], start=(hc == 0), stop=(hc == 1))
                k.op("scalar", "copy", out=kcmpT[:, g, 0:255], in_=pk[:, 0:255])
            else:
                for nt in range(2):
                    nn = 128
                    pk = C.psC
                    for hc in range(2):
                        k.op("tensor", "matmul", out=pk[0:nn, 0:64], lhsT=hid[:, hc, nt * 128:nt * 128 + nn], rhs=w2[:, hc, 0:64],
                             start=(hc == 0), stop=(hc == 1))
                    k.op("scalar", "copy", out=vcx[0:nn, nt, g, 0:64], in_=pk[0:nn, 0:64])
    k.end_phase()
    k.begin_phase()
    cst32 = k.sb("cst32", [128, 512], F32)

    def cload(name, shape, src):
        t_ = k.sb(name, shape, BF16)
        k.dma("sync", cst32[0:shape[0], 0:shape[1]], src)
        k.op("vector", "tensor_copy", out=t_[:], in_=cst32[0:shape[0], 0:shape[1]])
        return t_
    E = k.sb("E", [64, S], BF16)
    for j in range(8):
        k.dma("sync", cst32[0:64, :], C.cst['c_E'][:, j * 512:(j + 1) * 512])
        k.op("vector", "tensor_copy", out=E[:, j * 512:(j + 1) * 512], in_=cst32[0:64, :])
    causb = [cload("causb", [128, 512], C.cst['c_causb'][r]) for r in range(4)]
    winb = [cload("winb", [128, 512], C.cst['c_winb'][r]) for r in range(8)]
    cmpb = [cload("cmpb", [128, 512], C.cst['c_cmpb'][r]) for r in range(5)]
    qg_t = [k.sb("qgt", [64, 16, 512], BF16)] * 2
    pT_sel = k.sb("pT_sel", [128, 32, 512], BF16)
    pT_win = k.sb("pT_win", [128, 8, 512], BF16)
    pT_cmp = k.sb("pT_cmp", [128, 2, 512], BF16)
    outacc = k.sb("outacc", [128, 4, 1024], F32)
    imp = k.sb("imp", [128, 4, 64], F32)
    keep = k.sb("keep", [128, 4, 64], F32)
    addc = k.sb("addc", [128, 4, 64], F32)
    score = k.sb("score", [128, 64], F32)
    scr = k.sb("scr", [128, 64], F32)
    m8 = k.sb("m8", [128, 16], F32)
    sel = k.sb("sel", [128, 64], F32)
    val = k.sb("val", [128, 64], F32)
    negselT = k.sb("negselT", [64, 512], BF16)
    den = k.sb("den", [128, 4], F32)
    rot = [0]

    def branch(h, g, qg, kts, kmat, bias_fn, vmat_fn, acc_fn, pbuf):
        pb_ = 0
        qv = qg_t[qg % 2][:, h, :]
        for n_, kt in enumerate(kts):
            nn, klhs = kmat(kt, pb_)
            pS = C.psB[rot[0] % 3]
            rot[0] += 1
            extra = bias_fn(kt, nn)
            k.op("tensor", "matmul", out=pS[:, :], lhsT=klhs, rhs=qv, start=True, stop=(len(extra) == 0))
            for e_i, (l_, r_) in enumerate(extra):
                k.op("tensor", "matmul", out=pS[:, :], lhsT=l_, rhs=r_, start=False, stop=(e_i == len(extra) - 1))
            k.op("scalar", "activation", out=pbuf[:, n_, :], in_=pS[:, :], func=AF.Exp)
        for qi in range(4):
            for n_, kt in enumerate(kts):
                k.op("tensor", "matmul", out=acc_fn(qi), lhsT=pbuf[:, n_, qi * 128:(qi + 1) * 128], rhs=vmat_fn(kt, 128),
                     start=(n_ == 0), stop=(n_ == len(kts) - 1))

    def finish(h, qg, acc_fn, gcol, first):
        for qi in range(4):
            a_ = acc_fn(qi)
            qt = qg * 4 + qi
            k.op("vector", "tensor_scalar", out=den[:, 0:1], in0=a_[:, 64:65], scalar1=1e-30, scalar2=None, op0=ALU.max)
            k.op("vector", "reciprocal", out=den[:, 1:2], in_=den[:, 0:1])
            k.op("vector", "tensor_tensor", out=den[:, 2:3], in0=den[:, 1:2], in1=gates[:, qt, gcol:gcol + 1], op=ALU.mult)
            dst = outacc[:, qi, h * 64:(h + 1) * 64]
            if first:
                k.op("vector", "tensor_scalar", out=dst, in0=a_[:, 0:64], scalar1=den[:, 2:3], scalar2=None, op0=ALU.mult)
            else:
                k.op("vector", "scalar_tensor_tensor", out=dst, in0=a_[:, 0:64], scalar=den[:, 2:3], in1=dst, op0=ALU.mult, op1=ALU.add)
        return

    for qg in range(int(os.environ.get('NSA_NQG', '8'))):
        qt_ = qg_t[qg % 2]
        k.dma("sync", qt_[:], V(Qd, Qd.t[:, :, qg * 512:(qg + 1) * 512].rearrange("c (hh d) t -> d (c hh) t", hh=2)))
        k.dma("sync", keep[:], V(C.cst['c_selkeep'], C.cst['c_selkeep'].t[qg * 4:(qg + 1) * 4].rearrange("q p j -> p q j")))
        k.dma("sync", addc[:], V(C.cst['c_seladd'], C.cst['c_seladd'].t[qg * 4:(qg + 1) * 4].rearrange("q p j -> p q j")))
        for g in range(2):
            accC = lambda qi: C.psA[0][:, qi * 256:qi * 256 + 129]
            nts = [nt for nt in range(2) if qg - 4 * nt >= 0]
            for r in range(8):
                h = g * 8 + r
                branch(h, g, qg, nts,
                       lambda nt, pb_: (128, kcmpT[pb_:pb_ + 64, g, nt * 128:(nt + 1) * 128]),
                       lambda nt, nn: ([(C.identb[:], cmpb[qg - 4 * nt][:])] if qg - 4 * nt <= 4 else []),
                       lambda nt, nn: vcx[:, nt, g, 0:129], accC, pT_cmp)
                for qi in range(4):
                    a_ = accC(qi)
                    k.op("vector", "tensor_scalar", out=den[:, 0:1], in0=a_[:, 64:65], scalar1=1e-30, scalar2=None, op0=ALU.max)
                    k.op("vector", "reciprocal", out=den[:, 1:2], in_=den[:, 0:1])
                    if r == 0:
                        k.op("vector", "tensor_scalar", out=imp[:, qi, :], in0=a_[:, 65:129], scalar1=den[:, 1:2], scalar2=None, op0=ALU.mult)
                    else:
                        k.op("vector", "scalar_tensor_tensor", out=imp[:, qi, :], in0=a_[:, 65:129], scalar=den[:, 1:2], in1=imp[:, qi, :],
                             op0=ALU.mult, op1=ALU.add)
                finish(h, qg, accC, h * 3 + 0, True)
            for qi in range(4):
                k.op("vector", "tensor_tensor", out=score[:], in0=imp[:, qi, :], in1=keep[:, qi, :], op=ALU.mult)
                k.op("vector", "tensor_tensor", out=score[:], in0=score[:], in1=addc[:, qi, :], op=ALU.add)
                k.op("vector", "max", out=m8[:, 0:8], in_=score[:])
                k.op("vector", "match_replace", out=scr[:], in_to_replace=m8[:, 0:8], in_values=score[:], imm_value=-2e30)
                k.op("vector", "max", out=m8[:, 8:16], in_=scr[:])
                k.op("vector", "tensor_scalar", out=sel[:], in0=score[:], scalar1=m8[:, 15:16], scalar2=None, op0=ALU.is_ge)
                k.op("vector", "tensor_scalar", out=val[:], in0=score[:], scalar1=-5e29, scalar2=None, op0=ALU.is_gt)
                k.op("vector", "tensor_tensor", out=sel[:], in0=sel[:], in1=val[:], op=ALU.mult)
                k.op("vector", "tensor_scalar", out=sel[:], in0=sel[:], scalar1=1.0, scalar2=NSA_BIG, op0=ALU.subtract, op1=ALU.mult)
                k.op("tensor", "transpose", out=C.psC[0:64, 0:128], in_=sel[:], identity=C.ident[:])
                k.op("scalar", "copy", out=negselT[:, qi * 128:(qi + 1) * 128], in_=C.psC[0:64, 0:128])
            accS = lambda qi: C.psA[1][:, qi * 256:qi * 256 + 65]
            accW = lambda qi: C.psA[0][:, qi * 256 + 130:qi * 256 + 195]
            for r in range(0 if os.environ.get('NSA_BR') == 'c' else 8):
                h = g * 8 + r
                branch(h, g, qg, list(range(4 * qg + 4)),
                       lambda kt, pb_: (128, kT["ks"][g][pb_:pb_ + 64, kt * 128:(kt + 1) * 128]),
                       lambda kt, nn: [(E[:, kt * 128:(kt + 1) * 128], negselT[:, :])] + ([(C.identb[:], causb[kt - 4 * qg][:])] if kt >= 4 * qg else []),
                       lambda kt, nn: vx["vs"][:, kt, g, 0:65], accS, pT_sel)
                finish(h, qg, accS, h * 3 + 1, False)
                if os.environ.get('NSA_BR') == 'cs':
                    continue
                branch(h, g, qg, list(range(max(0, 4 * qg - 4), 4 * qg + 4)),
                       lambda kt, pb_: (128, kT["kw"][g][pb_:pb_ + 64, kt * 128:(kt + 1) * 128]),
                       lambda kt, nn: [(C.identb[:], winb[kt - 4 * qg + 4][:])],
                       lambda kt, nn: vx["vw"][:, kt, g, 0:65], accW, pT_win)
                finish(h, qg, accW, h * 3 + 2, False)
        k.dma("sync", V(Od, Od.t[qg * 512:(qg + 1) * 512, :].rearrange("(q p) d -> p q d", p=128)), outacc[:])
    k.end_phase()
    k.end_phase()
    k.end_phase()
    k.begin_phase()
    wo = k.sb("wo", [128, 8, 1024], BF16)
    wst2 = k.sb("wst2", [128, 4, 1024], F32)
    Wo = P['nsa_w_out']
    for q4 in range(2):
        load_cast(C, wo[:, q4 * 4:(q4 + 1) * 4, :], Wo, Wo.t[0, q4 * 512:(q4 + 1) * 512, :].rearrange("(k p) f -> p k f", p=128), wst2[:])
    G1 = load_bc(C, "G1", C.mod_d, C.mod_d.t[i, 2 * 1024:3 * 1024], plus1=True)
    lng = load_bc(C, "lng", P['ln1_g'], P['ln1_g'].t[i])
    lnb = load_bc(C, "lnb", P['ln1_b'], P['ln1_b'].t[i])
    xo = [k.sb("xo", [128, 1024], F32) for _ in range(2)]
    gin = [k.sb("gin", [128, 1024], F32) for _ in range(2)]
    gT = [k.sb("gT", [128, 8, 128], BF16) for _ in range(2)]
    tmps = ep_tmp(C, 2)
    for ti in range(NT):
        g_ = gin[ti % 2]
        k.dma("sync", g_[:], Od[ti * 128:(ti + 1) * 128, :])
        for g4 in range(2):
            bank = C.psB[g4 % 2]
            for c in range(4):
                cidx = g4 * 4 + c
                k.op("tensor", "transpose", out=bank[:, c * 128:(c + 1) * 128], in_=g_[:, cidx * 128:(cidx + 1) * 128], identity=C.ident[:])
            dstv = gT[ti % 2][:, g4 * 4:(g4 + 1) * 4, :].rearrange("p c t -> p (c t)")
            if g4 % 2 == 0:
                k.op("vector", "tensor_copy", out=dstv, in_=bank[:, :])
            else:
                k.op("scalar", "copy", out=dstv, in_=bank[:, :])
        ps = C.psA[ti % 2]
        for dh in range(2):
            for kc in range(8):
                k.op("tensor", "matmul", out=ps[:, dh * 512:(dh + 1) * 512], lhsT=gT[ti % 2][:, kc, :],
                     rhs=wo[:, kc, dh * 512:(dh + 1) * 512], start=(kc == 0), stop=(kc == 7))
        k.dma("sync", xo[ti % 2][:], xsrc[ti * 128:(ti + 1) * 128, :])
        epilogue(C, ps[:, :], xo[ti % 2], G1, lng, lnb, xdst[ti * 128:(ti + 1) * 128, :], tmps[ti % 2])
    k.end_phase()


def host_consts():
    cs = {}
    cs['c_ident'] = np.eye(128, dtype=np.float32)
    t = np.arange(S)
    cs['c_poolinv'] = np.stack([1.0 / np.minimum(t + 1, w) for w in (2, 4, 8, 16)]).astype(np.float32)
    cs['c_ropeinv_ret'] = (10000.0 ** (-np.arange(0, 256, 2, dtype=np.float64) / 256.0)).astype(np.float32).reshape(128, 1)
    ii = np.arange(128, dtype=np.float64)
    rel = ii[None, :] - ii[:, None]
    retD = np.zeros((4, 2, 128, 128), np.float64)
    for h in range(4):
        lg = np.log1p(-(2.0 ** (-5.0 - h)))
        retD[h, 0] = np.where(rel >= 0, np.exp(lg * np.maximum(rel, 0.0)), 0.0) / 16.0
        retD[h, 1] = np.exp(lg * (128.0 + rel)) / 16.0
    cs['c_retD'] = retD.astype(np.float32)
    p = np.arange(128)
    cs['c_ropeinv_nsa'] = (10000.0 ** (-(2.0 * (p % 32)) / 64.0)).astype(np.float32).reshape(128, 1)
    cs['c_ropesgn'] = np.where((p % 64) < 32, -1.0, 1.0).astype(np.float32).reshape(128, 1)
    BIG = 30000.0
    key = np.arange(128)[:, None]
    q = np.arange(512)[None, :]
    cs['c_E'] = (np.arange(S)[None, :] // 64 == np.arange(64)[:, None]).astype(np.float32)
    cs['c_causb'] = np.stack([np.where(128 * r + key <= q, 0.0, -BIG) for r in range(4)]).astype(np.float32)
    wb = []
    for ri in range(8):
        kp = 128 * (ri - 4) + key
        wb.append(np.where((kp <= q) & (kp > q - 512), 0.0, -BIG))
    cs['c_winb'] = np.stack(wb).astype(np.float32)
    cs['c_cmpb'] = np.stack([np.where(16 * key + 31 - q <= 512 * r, 0.0, -BIG) for r in range(5)]).astype(np.float32)
    tq = (np.arange(32)[:, None, None] * 128 + np.arange(128)[None, :, None])
    jj = np.arange(64)[None, None, :]
    cur = tq // 64
    valid = jj <= cur
    forced = (jj == 0) | (jj == cur) | (jj == cur - 1)
    cs['c_selkeep'] = (valid & ~forced).astype(np.float32)
    cs['c_seladd'] = np.where(~valid, -1e30, np.where(forced, 1e6, 0.0)).astype(np.float32)
    n = (np.arange(2)[:, None, None] * 128 + np.arange(128)[None, :, None])
    j2 = np.arange(64)[None, None, :]
    co = np.ones((128, 2, 1), np.float32)
    co[127, 1, 0] = 0.0
    cs['c_cmpones'] = co
    cs['c_ovl'] = ((n < 255) & (16 * n < (j2 + 1) * 64) & (16 * n + 32 > j2 * 64)).astype(np.float32)
    return cs


CONST_SPECS = {
    'c_ident': ((128, 128), F32),
    'c_poolinv': ((4, S), F32),
    'c_ropeinv_ret': ((128, 1), F32),
    'c_retD': ((4, 2, 128, 128), F32),
    'c_ropeinv_nsa': ((128, 1), F32),
    'c_ropesgn': ((128, 1), F32),
    'c_E': ((64, S), F32),
    'c_causb': ((4, 128, 512), F32),
    'c_winb': ((8, 128, 512), F32),
    'c_cmpb': ((5, 128, 512), F32),
    'c_selkeep': ((32, 128, 64), F32),
    'c_seladd': ((32, 128, 64), F32),
    'c_ovl': ((2, 128, 64), F32),
    'c_cmpones': ((128, 2, 1), F32),
}


def _copy_out(C, src):
    k = C.k
    k.begin_phase()
    t = [k.sb("cp", [128, 1024], F32) for _ in range(2)]
    for ti in range(NT):
        k.dma("sync", t[ti % 2][:], src[ti * 128:(ti + 1) * 128, :])
        k.dma("sync", C.out[ti * 128:(ti + 1) * 128, :], t[ti % 2][:])
    k.end_phase()


def build(layers=(0, 1, 2, 3), stop=None):
    nc = bass.Bass("TRN2", target_bir_lowering=False)
    es = ExitStack()
    with es:
        C = Ctx()
        C.k = K(nc, es)
        setup(C)
        phase_mod(C, layers)
        cur = C.x_in
        for n, i in enumerate(layers):
            if stop == 'mod':
                _copy_out(C, cur)
                break
            mid = C.xs[0]
            last = (n == len(layers) - 1)
            dst = C.out if last else C.xs[1]
            ph = {0: 'phase_ret', 1: 'phase_conv', 2: 'phase_nsa', 3: 'phase_pool'}[i % 4]
            globals()[ph](C, i, cur, mid)
            if stop == 'mix':
                _copy_out(C, mid)
                break
            phase_moe(C, i, mid, dst)
            cur = dst
        C.k.finish([C.out])
        print("instructions:", C.k.ninst, "sems:", C.k.nsem)
    return nc


_NC_CACHE = {}


def kernel(layers=(0, 1, 2, 3), stop=None, ncores=8, **inputs):
    key = (tuple(layers), stop)
    if key not in _NC_CACHE:
        _NC_CACHE[key] = build(layers, stop)
    nc = _NC_CACHE[key]
    cs = host_consts()
    x = np.asarray(inputs['x'])
    c = np.asarray(inputs['c'])
    pos = np.asarray(inputs['positions'])
    shared = {n: np.ascontiguousarray(np.asarray(inputs[n], dtype=np.float32)) for n in PARAM_SHAPES}
    shared.update(cs)
    in_maps = []
    for b in range(ncores):
        m = dict(shared)
        m['x'] = np.ascontiguousarray(x[b])
        m['c'] = np.ascontiguousarray(c[b])
        m['positions'] = np.ascontiguousarray(pos[b]).astype(np.int32)
        in_maps.append(m)
    res = run_bass_kernel_spmd(nc, in_maps, core_ids=list(range(ncores)))
    return np.stack([r['out'] for r in res.results], axis=0).astype(np.float32)
```
